# Optimizing a Trainium2 kernel written in Bass

```python
import math
import jax, jax.numpy as jnp
from jax import lax
import numpy as np

D_MODEL = 1024
BATCH = 8
SEQ = 2048
DEPTH = 4

CHUNK = 64
HEAD_DIM = 64
N_MIX_HEADS = D_MODEL // HEAD_DIM
MEM_HEADS = 4
SELF_HEADS = N_MIX_HEADS - MEM_HEADS
SWA_KV_HEADS = 3
SWA_GROUP = SELF_HEADS // SWA_KV_HEADS
WINDOW = 128
WINDOW_CHUNKS = WINDOW // CHUNK
ROT_DIM = HEAD_DIM // 4
ROT_HALF = ROT_DIM // 2
ROPE_THETA = 500000.0
FOX_BLOCK = 128
MEM_LEN = 256
D_FF = ((8 * D_MODEL // 3 + 127) // 128) * 128
N_EXPERTS = 8
TOP_K = 2
EXPERT_BLOCK = 128
LN_EPS = 1e-5
NEG = -1e30
ATTN_SCALE = HEAD_DIM ** -0.5
DN_ALPHA = (2.0 * DEPTH) ** 0.25
DN_BETA = (8.0 * DEPTH) ** -0.25
N_SWA_LAYERS = (DEPTH + 1) // 2
N_FOX_LAYERS = DEPTH // 2
SWA_IN = (SELF_HEADS + 2 * SWA_KV_HEADS + MEM_HEADS) * HEAD_DIM
FOX_IN = (3 * SELF_HEADS + MEM_HEADS) * HEAD_DIM + SELF_HEADS

kernel_name = "hybrid_swa_fox_memxattn_deepnorm_moe"


def layer_norm(x, g, b):
    xf = x.astype(jnp.float32)
    mu = jnp.mean(xf, axis=-1, keepdims=True)
    var = jnp.mean(jnp.square(xf - mu), axis=-1, keepdims=True)
    return ((xf - mu) * lax.rsqrt(var + LN_EPS) * g + b).astype(x.dtype)


def rope_tables(positions):
    inv_freq = ROPE_THETA ** (-jnp.arange(0, ROT_DIM, 2, dtype=jnp.float32) / ROT_DIM)
    ang = positions.astype(jnp.float32)[..., None] * inv_freq
    return jnp.cos(ang)[:, :, None, :], jnp.sin(ang)[:, :, None, :]


def partial_rope(t, cos, sin):
    r = t[..., :ROT_DIM].astype(jnp.float32)
    r1, r2 = r[..., :ROT_HALF], r[..., ROT_HALF:]
    rot = jnp.concatenate([r1 * cos - r2 * sin, r2 * cos + r1 * sin], axis=-1)
    return jnp.concatenate([rot.astype(t.dtype), t[..., ROT_DIM:]], axis=-1)


def swa_sink_attention(q, k, v, sinks):
    B, S = q.shape[0], q.shape[1]
    n_c = S // CHUNK
    pad = WINDOW_CHUNKS * CHUNK
    n_keys = (WINDOW_CHUNKS + 1) * CHUNK
    qb = q.reshape(B, n_c, CHUNK, SWA_KV_HEADS, SWA_GROUP, HEAD_DIM)

    def band(t):
        tp = jnp.pad(t, ((0, 0), (pad, 0), (0, 0), (0, 0)))
        tp = tp.reshape(B, n_c + WINDOW_CHUNKS, CHUNK, SWA_KV_HEADS, HEAD_DIM)
        return jnp.concatenate([tp[:, m:m + n_c] for m in range(WINDOW_CHUNKS + 1)], axis=2)

    kb, vb = band(k), band(v)
    s = jnp.einsum('bnqhgd,bnkhd->bnhgqk', qb, kb).astype(jnp.float32) * ATTN_SCALE
    key_chunk = jnp.arange(n_c)[:, None] - WINDOW_CHUNKS + (jnp.arange(n_keys) // CHUNK)[None, :]
    valid = key_chunk >= 0
    s = jnp.where(valid[None, :, None, None, None, :], s, NEG)
    sk = sinks.astype(jnp.float32).reshape(SWA_KV_HEADS, SWA_GROUP)[None, None, :, :, None]
    m = jnp.maximum(jnp.max(s, axis=-1), sk)
    p = jnp.exp(s - m[..., None])
    den = jnp.sum(p, axis=-1) + jnp.exp(sk - m)
    probs = (p / den[..., None]).astype(v.dtype)
    o = jnp.einsum('bnhgqk,bnkhd->bnqhgd', probs, vb)
    return o.reshape(B, S, SELF_HEADS * HEAD_DIM)


def forgetting_attention(q, k, v, log_f):
    B, S = q.shape[0], q.shape[1]
    cum = jnp.cumsum(log_f, axis=1).transpose(0, 2, 1)
    outs = []
    for i in range(S // FOX_BLOCK):
        lo, hi = i * FOX_BLOCK, (i + 1) * FOX_BLOCK
        s = jnp.einsum('bqhd,bkhd->bhqk', q[:, lo:hi], k[:, :hi]).astype(jnp.float32) * ATTN_SCALE
        bias = cum[:, :, lo:hi, None] - cum[:, :, None, :hi]
        causal = jnp.arange(lo, hi)[:, None] >= jnp.arange(hi)[None, :]
        s = jnp.where(causal, s + bias, NEG)
        p = jax.nn.softmax(s, axis=-1).astype(v.dtype)
        outs.append(jnp.einsum('bhqk,bkhd->bqhd', p, v[:, :hi]))
    return jnp.concatenate(outs, axis=1).reshape(B, S, SELF_HEADS * HEAD_DIM)


def memory_attention(qc, km, vm):
    B, S = qc.shape[0], qc.shape[1]
    s = jnp.einsum('bshd,bmhd->bhsm', qc, km).astype(jnp.float32) * ATTN_SCALE
    p = jax.nn.softmax(s, axis=-1).astype(vm.dtype)
    return jnp.einsum('bhsm,bmhd->bshd', p, vm).reshape(B, S, MEM_HEADS * HEAD_DIM)


def swiglu(x, w_in, w_out):
    h = x @ w_in
    a, b = h[..., :D_FF], h[..., D_FF:]
    return (jax.nn.silu(a) * b) @ w_out


def moe_swiglu(x, w_router, b_router, w_in, w_out):
    shp = x.shape
    x2 = x.reshape(-1, shp[-1])
    n_tok = x2.shape[0]
    logits = (x2 @ w_router).astype(jnp.float32) + b_router.astype(jnp.float32)
    top_vals, top_idx = lax.top_k(logits, TOP_K)
    gates = jax.nn.softmax(top_vals, axis=-1)
    n_asg = n_tok * TOP_K
    e_flat = top_idx.reshape(-1)
    t_flat = jnp.repeat(jnp.arange(n_tok, dtype=jnp.int32), TOP_K)
    g_flat = gates.reshape(-1)
    order = jnp.argsort(e_flat)
    se, st, sg = e_flat[order], t_flat[order], g_flat[order]
    counts = jnp.bincount(e_flat, length=N_EXPERTS)
    starts = jnp.cumsum(counts) - counts
    pcounts = (counts + EXPERT_BLOCK - 1) // EXPERT_BLOCK * EXPERT_BLOCK
    pends = jnp.cumsum(pcounts)
    pstarts = pends - pcounts
    dest = pstarts[se] + (jnp.arange(n_asg) - starts[se])
    n_blocks = -(-n_asg // EXPERT_BLOCK) + N_EXPERTS
    n_slots = n_blocks * EXPERT_BLOCK
    slot_tok = jnp.zeros((n_slots,), jnp.int32).at[dest].set(st)
    slot_gate = jnp.zeros((n_slots,), x2.dtype).at[dest].set(sg.astype(x2.dtype))
    block_exp = jnp.minimum(
        jnp.searchsorted(pends, jnp.arange(n_blocks) * EXPERT_BLOCK, side='right'), N_EXPERTS - 1)
    xs = x2[slot_tok].reshape(n_blocks, EXPERT_BLOCK, shp[-1])

    def expert_block(args):
        xb, e = args
        h = xb @ w_in[e]
        return (jax.nn.silu(h[:, :D_FF]) * h[:, D_FF:]) @ w_out[e]

    ys = lax.map(expert_block, (xs, block_exp)).reshape(n_slots, shp[-1])
    y = jnp.zeros_like(x2).at[slot_tok].add((ys * slot_gate[:, None]).astype(x2.dtype))
    return y.reshape(shp)


def setup_inputs(seed: int = 0) -> dict:
    key = jax.random.key(seed)
    ks = jax.random.split(key, 20)
    nrm = jax.random.normal
    f32 = jnp.float32
    x = nrm(ks[0], (BATCH, SEQ, D_MODEL), f32)
    mem = nrm(ks[1], (BATCH, MEM_LEN, D_MODEL), f32)
    offset = jax.random.randint(ks[2], (BATCH, 1), 0, 1 << 16, dtype=jnp.int32)
    positions = offset + jnp.arange(SEQ, dtype=jnp.int32)[None, :]
    sd = D_MODEL ** -0.5
    sf = D_FF ** -0.5
    return {
        "x": x,
        "mem": mem,
        "positions": positions,
        "w_in_swa": nrm(ks[3], (N_SWA_LAYERS, D_MODEL, SWA_IN), f32) * sd,
        "attn_sinks": nrm(ks[4], (N_SWA_LAYERS, SELF_HEADS), f32) * 0.5,
        "w_in_fox": nrm(ks[5], (N_FOX_LAYERS, D_MODEL, FOX_IN), f32) * sd,
        "b_forget": jax.random.uniform(ks[6], (N_FOX_LAYERS, SELF_HEADS), f32, 1.0, 4.0),
        "w_mem_kv": nrm(ks[7], (DEPTH, D_MODEL, 2 * MEM_HEADS * HEAD_DIM), f32) * sd,
        "w_out": nrm(ks[8], (DEPTH, D_MODEL, D_MODEL), f32) * sd * DN_BETA,
        "ln_attn_g": 1.0 + 0.02 * nrm(ks[9], (DEPTH, D_MODEL), f32),
        "ln_attn_b": 0.02 * nrm(ks[10], (DEPTH, D_MODEL), f32),
        "w_ffn_in": nrm(ks[11], (N_SWA_LAYERS, D_MODEL, 2 * D_FF), f32) * sd,
        "w_ffn_out": nrm(ks[12], (N_SWA_LAYERS, D_FF, D_MODEL), f32) * sf * DN_BETA,
        "w_router": nrm(ks[13], (N_FOX_LAYERS, D_MODEL, N_EXPERTS), f32) * sd,
        "b_router": 0.01 * nrm(ks[14], (N_FOX_LAYERS, N_EXPERTS), f32),
        "w_exp_in": nrm(ks[15], (N_FOX_LAYERS, N_EXPERTS, D_MODEL, 2 * D_FF), f32) * sd,
        "w_exp_out": nrm(ks[16], (N_FOX_LAYERS, N_EXPERTS, D_FF, D_MODEL), f32) * sf * DN_BETA,
        "ln_ffn_g": 1.0 + 0.02 * nrm(ks[17], (DEPTH, D_MODEL), f32),
        "ln_ffn_b": 0.02 * nrm(ks[18], (DEPTH, D_MODEL), f32),
    }


def reference(x, mem, positions, w_in_swa, attn_sinks, w_in_fox, b_forget, w_mem_kv, w_out,
              ln_attn_g, ln_attn_b, w_ffn_in, w_ffn_out, w_router, b_router, w_exp_in,
              w_exp_out, ln_ffn_g, ln_ffn_b):
    B, S = x.shape[0], x.shape[1]
    cos, sin = rope_tables(positions)
    hq = SELF_HEADS * HEAD_DIM
    hkv = SWA_KV_HEADS * HEAD_DIM
    hm = MEM_HEADS * HEAD_DIM
    for i in range(DEPTH):
        j = i // 2
        kvm = mem @ w_mem_kv[i]
        km = kvm[..., :hm].reshape(B, MEM_LEN, MEM_HEADS, HEAD_DIM)
        vm = kvm[..., hm:].reshape(B, MEM_LEN, MEM_HEADS, HEAD_DIM)
        if i % 2 == 0:
            proj = x @ w_in_swa[j]
            q = proj[..., :hq].reshape(B, S, SELF_HEADS, HEAD_DIM)
            k = proj[..., hq:hq + hkv].reshape(B, S, SWA_KV_HEADS, HEAD_DIM)
            v = proj[..., hq + hkv:hq + 2 * hkv].reshape(B, S, SWA_KV_HEADS, HEAD_DIM)
            qc = proj[..., hq + 2 * hkv:].reshape(B, S, MEM_HEADS, HEAD_DIM)
            q, k = partial_rope(q, cos, sin), partial_rope(k, cos, sin)
            self_out = swa_sink_attention(q, k, v, attn_sinks[j])
        else:
            proj = x @ w_in_fox[j]
            q = proj[..., :hq].reshape(B, S, SELF_HEADS, HEAD_DIM)
            k = proj[..., hq:2 * hq].reshape(B, S, SELF_HEADS, HEAD_DIM)
            v = proj[..., 2 * hq:3 * hq].reshape(B, S, SELF_HEADS, HEAD_DIM)
            qc = proj[..., 3 * hq:3 * hq + hm].reshape(B, S, MEM_HEADS, HEAD_DIM)
            f_logit = proj[..., 3 * hq + hm:].astype(jnp.float32) + b_forget[j].astype(jnp.float32)
            log_f = jax.nn.log_sigmoid(f_logit)
            self_out = forgetting_attention(q, k, v, log_f)
        cross_out = memory_attention(qc, km, vm)
        mix = jnp.concatenate([self_out, cross_out], axis=-1) @ w_out[i]
        x = layer_norm(DN_ALPHA * x + mix, ln_attn_g[i], ln_attn_b[i])
        if i % 2 == 0:
            ffn = swiglu(x, w_ffn_in[j], w_ffn_out[j])
        else:
            ffn = moe_swiglu(x, w_router[j], b_router[j], w_exp_in[j], w_exp_out[j])
        x = layer_norm(DN_ALPHA * x + ffn, ln_ffn_g[i], ln_ffn_b[i])
    return x
```

```python
import numpy as np
from contextlib import ExitStack
import concourse.bass as bass
import concourse.mybir as mybir
from concourse.bass_utils import run_bass_kernel_spmd

F32 = mybir.dt.float32
BF16 = mybir.dt.bfloat16
I32 = mybir.dt.int32
AF = mybir.ActivationFunctionType
ALU = mybir.AluOpType

S = 2048
D = 1024
NCH = 8
NTG = 4
NTB = 16
DFF = 2816
NFC = 22
ALPHA = 8.0 ** 0.25
SCALE = 0.125
EPOCH = 12000
PI = float(np.pi)


class Buf:
    __slots__ = ("name", "lw", "rd", "phase")

    def __init__(self, name, phase=None):
        self.name = name
        self.lw = None
        self.rd = []
        self.phase = phase


class SemGrp:
    __slots__ = ("sem", "cnt", "batch", "idx")

    def __init__(self, batch=False):
        self.sem = None
        self.cnt = 0
        self.batch = batch


class Op:
    __slots__ = ("eng", "fn", "deps", "sig", "grp", "count", "dma", "epoch")


class Prog:
    def __init__(self):
        self.ops = []
        self.grps = []

    def grp(self, batch=False):
        g = SemGrp(batch)
        self.grps.append(g)
        return g

    def add(self, eng, fn, reads=(), writes=(), dma=None):
        op = Op()
        op.eng = eng
        op.fn = fn
        op.dma = dma is not None
        op.grp = dma
        op.sig = op.dma
        op.count = 0
        op.epoch = 0
        reads = list(reads)
        writes = list(writes)
        for b in list(reads) + list(writes):
            if b.phase is not None:
                reads.append(b.phase)
        deps = {}
        for b in reads:
            if b.lw is not None:
                deps[id(b.lw)] = (b.lw, True)
        for b in writes:
            if b.lw is not None and id(b.lw) not in deps:
                deps[id(b.lw)] = (b.lw, False)
            for r in b.rd:
                if id(r) not in deps:
                    deps[id(r)] = (r, False)
        fin = []
        for p, raw in deps.values():
            if p is op:
                continue
            if op.dma and p.dma and p.grp is op.grp and op.grp.batch:
                continue
            if (not op.dma) and (not p.dma) and p.eng == eng:
                if eng == "pe":
                    continue
            fin.append(p)
            p.sig = True
        op.deps = fin
        for b in reads:
            if not op.dma:
                b.rd = [r for r in b.rd if r.dma or r.eng != eng]
            b.rd.append(op)
        for b in writes:
            b.lw = op
            b.rd = []
        self.ops.append(op)
        return op

    def emit(self, nc, es):
        engs = ["pe", "act", "dve", "pool", "sp"]
        ecount = {e: 0 for e in engs}
        eepoch = {e: 0 for e in engs}
        for op in self.ops:
            if op.dma:
                op.grp.cnt += 16
                op.count = op.grp.cnt
            elif op.sig:
                if ecount[op.eng] >= EPOCH:
                    ecount[op.eng] = 0
                    eepoch[op.eng] += 1
                ecount[op.eng] += 1
                op.count = ecount[op.eng]
                op.epoch = eepoch[op.eng]
        for gi, g in enumerate(self.grps):
            if g.cnt > 0:
                g.sem = es.enter_context(nc.semaphore(f"sg{gi}"))
        esem = {}
        for e in engs:
            for k in range(eepoch[e] + 1):
                esem[(e, k)] = es.enter_context(nc.semaphore(f"se_{e}_{k}"))
        for op in self.ops:
            if op.dma and op.grp.batch:
                op.count = op.grp.cnt
        block = es.enter_context(nc.Block())
        streams = {e: [o for o in self.ops if o.eng == e] for e in engs}

        def run(engname, eobj):
            waited = {}
            for op in streams[engname]:
                need = {}
                for p in op.deps:
                    if p.dma:
                        key = ("g", id(p.grp))
                        sem = p.grp.sem
                    else:
                        key = (p.eng, p.epoch)
                        sem = esem[key]
                    if waited.get(key, 0) >= p.count:
                        continue
                    if key not in need or need[key][1] < p.count:
                        need[key] = (sem, p.count)
                for key, (sem, cnt) in need.items():
                    eobj.wait_ge(sem, cnt)
                    waited[key] = cnt
                ins = op.fn(eobj)
                if ins is None:
                    continue
                if op.dma:
                    ins.then_inc(op.grp.sem, 16)
                elif op.sig:
                    ins.then_inc(esem[(op.eng, op.epoch)], 1)

        if streams["pe"]:
            @block.tensor
            def _(e):
                run("pe", e)

        if streams["act"]:
            @block.scalar
            def _(e):
                run("act", e)

        if streams["dve"]:
            @block.vector
            def _(e):
                run("dve", e)

        if streams["pool"]:
            @block.gpsimd
            def _(e):
                run("pool", e)

        if streams["sp"]:
            @block.sync
            def _(e):
                run("sp", e)


class Rot:
    def __init__(self, items):
        self.items = items
        self.i = 0

    def next(self):
        it = self.items[self.i % len(self.items)]
        self.i += 1
        return it


NCP = 128 + 128 + 128 + 3
CP_ID, CP_ONESM, CP_ONES, CP_INVF = 0, 128, 256, 384
CP_SIGN, CP_EPS = CP_INVF + 1, CP_INVF + 2
NCX = 128 + 256 + 12 * 128 + 512
CX_TRI, CX_SWAM, CX_SEL, CX_ONES = 0, 128, 384, 384 + 1536
NSP = 128 + 24 + 16 + 2 + 128 + 192
SP_LN, SP_SINK, SP_BR, SP_BF, SP_WR, SP_WF = 0, 128, 152, 168, 170, 298
SWA_EXT = 2752


def _swap16(d):
    return d + 8 if d < 8 else (d - 8 if d < 16 else d)


def _make_consts():
    cp = np.zeros((128, NCP), np.float32)
    cx = np.zeros((128, NCX), np.float32)
    cp[:, CP_ID:CP_ID + 128] = np.eye(128, dtype=np.float32)
    cp[:, CP_ONESM:CP_ONESM + 128] = 1.0 / 1024.0
    k = np.arange(128)[:, None]
    q = np.arange(128)[None, :]
    cx[:, CX_TRI:CX_TRI + 128] = (q >= k).astype(np.float32)
    mp = np.ones((128, 128), np.float32)
    mp[:64, 64:] = 0.0
    mo = np.ones((128, 128), np.float32)
    mo[64:, :64] = 0.0
    cx[:, CX_SWAM:CX_SWAM + 128] = mp
    cx[:, CX_SWAM + 128:CX_SWAM + 256] = mo
    cp[:, CP_ONES:CP_ONES + 128] = 1.0
    cx[:, CX_ONES:CX_ONES + 512] = 1.0
    for h in range(12):
        cx[h, CX_SEL + h * 128:CX_SEL + (h + 1) * 128] = 1.0
        cx[64 + h, CX_SEL + h * 128:CX_SEL + (h + 1) * 128] = 1.0
    inv_freq = (500000.0 ** (-np.arange(0, 16, 2, dtype=np.float32) / 16)).astype(np.float32)
    for p in range(128):
        d = p % 64
        if d < 16:
            cp[p, CP_INVF] = inv_freq[d % 8]
            cp[p, CP_SIGN] = -1.0 if d < 8 else 1.0
    cp[:, CP_EPS] = 1e-5
    return cp, cx


def _make_sp(inp):
    sp = np.zeros((128, NSP), np.float32)
    vecs = [inp["ln_attn_g"], inp["ln_attn_b"], inp["ln_ffn_g"], inp["ln_ffn_b"]]
    for l in range(4):
        for j in range(4):
            sp[:, SP_LN + (l * 4 + j) * 8:SP_LN + (l * 4 + j) * 8 + 8] = vecs[j][l].reshape(8, 128).T
    sp[:, SP_SINK:SP_SINK + 24] = inp["attn_sinks"].reshape(1, 24)
    sp[:, SP_BR:SP_BR + 16] = inp["b_router"].reshape(1, 16)
    sp[:12, SP_BF:SP_BF + 2] = inp["b_forget"].T
    for j in range(2):
        sp[:, SP_WR + j * 64:SP_WR + (j + 1) * 64] = (
            inp["w_router"][j].reshape(8, 128, 8).transpose(1, 0, 2).reshape(128, 64))
        sp[:, SP_WF + j * 96:SP_WF + (j + 1) * 96] = (
            inp["w_in_fox"][j][:, 2560:2572].reshape(8, 128, 12).transpose(1, 0, 2).reshape(128, 96))
    return sp


def _swa_cols():
    main = list(range(768))
    for g in range(3):
        for r in range(2):
            main += [768 + g * 64 + d for d in range(64)]
    main += list(range(960, 1408))
    sw = []
    for h in range(12):
        sw += [h * 64 + _swap16(d) for d in range(64)]
    for g in range(3):
        for r in range(2):
            sw += [768 + g * 64 + _swap16(d) for d in range(64)]
    return np.array(main + sw, np.int64)


def build(n_layers=4, taps=()):
    import os as _os
    nc = bass.Bass("TRN2", target_bir_lowering=False)
    pr = Prog()
    es = ExitStack()

    def din(name, shape, dt=F32):
        return nc.dram_tensor(name, list(shape), dt, kind="ExternalInput").ap()

    x_d = din("x", [S, D])
    mem_d = din("mem", [256, D])
    pos_d = din("pos", [1, S], I32)
    if n_layers >= 1:
        wswa_d = din("w_swa", [2, D, SWA_EXT])
        wkv_d = din("w_kv", [4, D, 512])
        wout_d = din("w_out", [4, D, D])
        wfi_d = din("w_ffn_in", [2, D, 2 * DFF])
        wfo_d = din("w_ffn_out", [2, DFF, D])
    if n_layers >= 2:
        wfox_d = din("w_fox", [2, D, 2572])
        wei_d = din("w_exp_in", [2, 8, D, 2 * DFF])
        weo_d = din("w_exp_out", [2, 8, DFF, D])
    sp_d = din("sp", [128, NSP])
    cp_d = din("cp", [128, NCP])
    cx_d = din("cx", [128, NCX])
    out_d = nc.dram_tensor("out", [S, D], F32, kind="ExternalOutput").ap()
    tap_d = {}
    for name, shape in taps:
        tap_d[name] = nc.dram_tensor(name, list(shape), F32, kind="ExternalOutput").ap()

    def sb(name, shape, dt):
        return es.enter_context(nc.sbuf_tensor("s_" + name, list(shape), dt))

    xres = sb("xres", [128, NCH, S], F32)
    xb = sb("xb", [128, NCH, S], BF16)
    arena = sb("arena", [128, 25600], BF16)
    wbase = [sb(f"wbase{i}", [128, 4096], BF16) for i in range(2)]
    Ctab = sb("Ctab", [128, S], BF16)
    Stab = sb("Stab", [128, S], BF16)
    cp = sb("cp", [128, NCP], F32)
    spm = sb("spm", [128, NSP], F32)
    lnps = sb("lnps", [128, 128], F32)
    sinkE = sb("sinkE", [128, 24], F32)
    negbf = sb("negbf", [128, 2], F32)
    tri_bf = sb("tri_bf", [128, 128], BF16)
    swam_bf = sb("swam_bf", [128, 256], BF16)
    ones_bf = sb("ones_bf", [128, 512], BF16)
    sel_bf = sb("sel_bf", [128, 1536], BF16)
    memT = sb("memT", [128, NCH, 256], BF16)
    kmT = sb("kmT", [128, 2, 256], BF16)
    vm = sb("vm", [128, 2, 256], BF16)
    negD = sb("negD", [128, NTB, 12], F32)
    Gt = sb("Gt", [128, NTB, 8], F32)
    tmpf = [sb(f"tmpf{i}", [128, 512], F32) for i in range(6)]
    onesm_bf = sb("onesm_bf", [128, 128], BF16)
    ln_mean = sb("ln_mean", [128, 512], F32)
    ln_rstd = sb("ln_rstd", [128, 512], F32)
    B_lnm = Buf("ln_mean")
    B_lnr = Buf("ln_rstd")
    ptb = [sb(f"pt{i}", [128, 512], BF16) for i in range(4)]
    smallf = [sb(f"small{i}", [128, 16], F32) for i in range(8)]
    ps = [es.enter_context(nc.psum_tensor(f"p_ps{i}", [128, 512], F32)) for i in range(8)]

    B_xres = [[Buf(f"xres{c}_{t}") for t in range(NTG)] for c in range(NCH)]
    B_xb = [[Buf(f"xb{c}_{t}") for t in range(NTG)] for c in range(NCH)]
    B_phase = Buf("phase")
    B_ps = [Buf(f"ps{i}") for i in range(8)]
    B_tmpf = [Buf(f"tmpf{i}") for i in range(6)]
    B_pt = [Buf(f"pt{i}") for i in range(4)]
    B_small = [Buf(f"small{i}") for i in range(8)]
    B_wbase = [Buf(f"wbase{i}") for i in range(2)]
    G_wbase = [pr.grp() for _ in range(2)]
    B_const = Buf("const")
    B_tab = Buf("tab")
    B_memT = Buf("memT")
    B_kmT = Buf("kmT")
    B_vm = Buf("vm")
    B_negD = [Buf(f"negD{t}") for t in range(NTG)]
    B_Gt = [Buf(f"Gt{t}") for t in range(NTB)]
    B_out = Buf("outdram")
    G_const = pr.grp(batch=True)

    tmp_rot = Rot(list(zip(tmpf, B_tmpf)))
    pt_rot = Rot(list(zip(ptb, B_pt)))
    small_rot = Rot(list(zip(smallf, B_small)))

    def tgs(t):
        return slice(t * 512, (t + 1) * 512)

    def tbs(t):
        return slice(t * 128, (t + 1) * 128)

    def mm(out, lhsT, rhs, start, stop, reads, writes):
        pr.add("pe", lambda e: e.matmul(out, lhsT, rhs, start=start, stop=stop), reads, writes)

    def tr(out, in_, ident, reads, writes):
        pr.add("pe", lambda e: e.transpose(out, in_, ident), reads, writes)

    def act(out, in_, func, reads, writes, bias=None, scale=None):
        kw = {}
        if bias is not None:
            kw["bias"] = bias
        if scale is not None:
            kw["scale"] = scale
        pr.add("act", lambda e: e.activation(out, in_, func, **kw), reads, writes)

    def vtt(out, a, b, op, reads, writes):
        pr.add("dve", lambda e: e.tensor_tensor(out, a, b, op), reads, writes)

    def vts(out, a, s1, s2, op0, op1, reads, writes):
        if op1 is None:
            pr.add("dve", lambda e: e.tensor_scalar(out, a, s1, None, op0), reads, writes)
        else:
            pr.add("dve", lambda e: e.tensor_scalar(out, a, s1, s2, op0, op1), reads, writes)

    def vcopy(out, a, reads, writes):
        pr.add("dve", lambda e: e.tensor_copy(out, a), reads, writes)

    def vrecip(out, a, reads, writes):
        pr.add("dve", lambda e: e.reciprocal(out, a), reads, writes)

    def dma_sp(out, in_, grp, reads, writes):
        pr.add("sp", lambda e: e.dma_start(out=out, in_=in_), reads, writes, dma=grp)

    def dma_pool(out, in_, grp, reads, writes):
        pr.add("pool", lambda e: e.dma_start(out=out, in_=in_), reads, writes, dma=grp)

    def phase_switch():
        sm, bsm = small_rot.next()
        pr.add("dve", lambda e: e.memset(sm[:, 0:1], 0.0), [], [bsm, B_phase])

    def aview(off, n):
        return arena[:, off:off + n]

    QA_OFF = 0
    KV_OFF = 16384
    qa = aview(QA_OFF, 16384).rearrange("p (c t) -> p c t", c=NCH)
    B_qa = [[Buf(f"qa{c}_{t}", B_phase) for t in range(NTG)] for c in range(NCH)]
    gp = aview(0, 8192).rearrange("p (c t) -> p c t", c=4)
    B_gp = [[Buf(f"gp{c}_{t}", B_phase) for t in range(NTG)] for c in range(4)]
    GB = aview(8192, 4096).bitcast(F32)
    B_GB = [Buf(f"GB{t}", B_phase) for t in range(NTG)]
    wext = [aview(12288 + i * 4096, 4096) for i in range(3)]
    B_wext = [Buf(f"wext{i}", B_phase) for i in range(3)]
    G_wext = [pr.grp() for _ in range(3)]
    stg = [aview(i * 2048, 2048).bitcast(F32) for i in range(4)]
    if _os.environ.get('DED_STG'):
        stg = [tmpf[i][:, :] for i in range(2)] * 2
        stg = [sb(f"stgd{i}", [128, 1024], F32)[:, :] for i in range(4)]
    B_stg = [Buf(f"stg{i}", B_phase) for i in range(4)]
    G_stg = [pr.grp() for _ in range(4)]

    w_attn_rot = Rot([(wbase[i][:, :], B_wbase[i], G_wbase[i]) for i in range(2)])
    w_ffn_rot = Rot([(wbase[i][:, :], B_wbase[i], G_wbase[i]) for i in range(2)]
                    + [(wext[i], B_wext[i], G_wext[i]) for i in range(3)])

    def wload_in(rot, src2d, col_list):
        wt, bw, gw = rot.next()
        ntot = sum(n for _, n in col_list)
        v = wt[:, 0:8 * ntot].rearrange("p (k n) -> p k n", k=8)
        o = 0
        for c0, n in col_list:
            dma_pool(v[:, :, o:o + n], src2d[:, c0:c0 + n].rearrange("(k p) n -> p k n", p=128), gw, [], [bw])
            o += n
        return v, bw

    def wload_rows(rot, src2d, r0, nchunks, ncols):
        wt, bw, gw = rot.next()
        v = wt[:, 0:nchunks * ncols].rearrange("p (k n) -> p k n", k=nchunks)
        dma_pool(v, src2d[r0 * 128:(r0 + nchunks) * 128, :].rearrange("(k p) n -> p k n", p=128), gw, [], [bw])
        return v, bw

    ident = cp[:, CP_ID:CP_ID + 128]
    onesm = cp[:, CP_ONESM:CP_ONESM + 128]
    eps_ap = cp[:, CP_EPS:CP_EPS + 1]

    import os as _os
    dma_sp(cp[:, :], cp_d, G_const, [], [B_const])
    dma_sp(spm[:, :], sp_d, G_const, [], [B_const])
    if _os.environ.get('SKIP_CX'):
        cx_d = None
    cxs = aview(8192, 2 * NCX).bitcast(F32)
    B_cxs = Buf("cxs", B_phase)
    G_cxs = pr.grp()
    if cx_d is not None:
        dma_sp(cxs, cx_d, G_cxs, [], [B_cxs])
        vcopy(tri_bf[:, :], cxs[:, CX_TRI:CX_TRI + 128], [B_cxs], [B_tab])
        vcopy(swam_bf[:, :], cxs[:, CX_SWAM:CX_SWAM + 256], [B_cxs], [B_tab])
        vcopy(ones_bf[:, :], cxs[:, CX_ONES:CX_ONES + 512], [B_cxs], [B_tab])
        vcopy(sel_bf[:, :], cxs[:, CX_SEL:CX_SEL + 1536], [B_cxs], [B_tab])
    vts(lnps[:, :], spm[:, SP_LN:SP_LN + 128], ALPHA, None, ALU.mult, None, [B_const], [B_tab])
    lastl = n_layers - 1
    if lastl >= 0:
        c0 = (lastl * 4 + 2) * 8
        vcopy(lnps[:, c0:c0 + 16], spm[:, SP_LN + c0:SP_LN + c0 + 16], [B_const, B_tab], [B_tab])
    if not _os.environ.get("SKIP_SINK"):
        act(sinkE[:, :], spm[:, SP_SINK:SP_SINK + 24], AF.Exp, [B_const], [B_tab])
    vts(negbf[:, :], spm[:, SP_BF:SP_BF + 2], -1.0, None, ALU.mult, None, [B_const], [B_tab])
    vcopy(onesm_bf[:, :], cp[:, CP_ONESM:CP_ONESM + 128], [B_const], [B_tab])

    import os as _os
    G_pos = pr.grp()
    for t in range(NTG if not _os.environ.get("SKIP_ROPE") else 0):
        pt_, bpt = tmpf[0], B_tmpf[0]
        pi_ = pt_[:, :].bitcast(I32)
        dma_sp(pi_, pos_d[0:1, tgs(t)].partition_broadcast(128), G_pos, [], [bpt])
        pf, bpf = tmpf[1], B_tmpf[1]
        vcopy(pf[:, :], pi_, [bpt], [bpf])
        ang, bang = tmpf[2], B_tmpf[2]
        vts(ang[:, :], pf[:, :], cp[:, CP_INVF:CP_INVF + 1], None, ALU.mult, None, [bpf, B_const], [bang])
        for which in range(2):
            a2, ba2 = tmpf[3], B_tmpf[3]
            if which == 1:
                vts(a2[:, :], ang[:, :], PI / 2, None, ALU.add, None, [bang], [ba2])
            else:
                vcopy(a2[:, :], ang[:, :], [bang], [ba2])
            u, bu = tmpf[4], B_tmpf[4]
            vts(u[:, :], a2[:, :], 1.0 / (2 * PI), 0.5, ALU.mult, ALU.add, [ba2], [bu])
            ui = pt_[:, :].bitcast(I32)
            vcopy(ui, u[:, :], [bu], [bpt])
            vcopy(u[:, :], ui, [bpt], [bu])
            C1, C2, C3 = 6.28125, 0.0019350051879882812, 3.019916050561005e-07
            for cc in (C1, C2, C3):
                pr.add("dve", (lambda cc_: lambda e: e.scalar_tensor_tensor(a2[:, :], u[:, :], -cc_, a2[:, :], ALU.mult, ALU.add))(cc),
                       [bu, ba2], [ba2])
            m, bm = tmpf[5], B_tmpf[5]
            vts(m[:, :], a2[:, :], PI, -2 * PI, ALU.is_gt, ALU.mult, [ba2], [bm])
            vtt(a2[:, :], a2[:, :], m[:, :], ALU.add, [ba2, bm], [ba2])
            vts(m[:, :], a2[:, :], -PI, 2 * PI, ALU.is_lt, ALU.mult, [ba2], [bm])
            vtt(a2[:, :], a2[:, :], m[:, :], ALU.add, [ba2, bm], [ba2])
            vts(a2[:, :], a2[:, :], PI, -PI, ALU.min, ALU.max, [ba2], [ba2])
            if which == 1:
                act(Ctab[:, tgs(t)], a2[:, :], AF.Sin, [ba2], [B_tab])
            else:
                act(a2[:, :], a2[:, :], AF.Sin, [ba2], [ba2])
                vts(Stab[:, tgs(t)], a2[:, :], cp[:, CP_SIGN:CP_SIGN + 1], None, ALU.mult, None, [ba2, B_const], [B_tab])

    ps_rot = Rot([(ps[i], B_ps[i]) for i in range(4)])
    stg_t = [(tmpf[0], tmpf[1]), (tmpf[2], tmpf[3]), (tmpf[4], tmpf[5]), (ln_mean, ln_rstd)]
    stg_b = [(B_tmpf[0], B_tmpf[1]), (B_tmpf[2], B_tmpf[3]), (B_tmpf[4], B_tmpf[5]), (B_lnm, B_lnr)]
    G_st2 = [[pr.grp() for _ in range(2)] for _ in range(4)]
    for t in range(NTG):
        for j in range(4):
            for hf in range(2):
                dma_sp(stg_t[j][hf][:, :], x_d[tbs(t * 4 + j), hf * 512:(hf + 1) * 512], G_st2[j][hf], [], [stg_b[j][hf]])
        for c in range(NCH):
            pp, bpp = ps_rot.next()
            hf = c // 4
            for j in range(4):
                tr(pp[:, j * 128:(j + 1) * 128], stg_t[j][hf][:, (c % 4) * 128:(c % 4 + 1) * 128], ident,
                   [stg_b[j][hf], B_const], [bpp])
            act(xres[:, c, tgs(t)], pp[:, :], AF.Identity, [bpp], [B_xres[c][t]], scale=ALPHA)
            act(xb[:, c, tgs(t)], pp[:, :], AF.Identity, [bpp], [B_xb[c][t]])
    for j in range(2):
        for hf in range(2):
            dma_sp(stg_t[j][hf][:, :], mem_d[tbs(j), hf * 512:(hf + 1) * 512], G_st2[j][hf], [], [stg_b[j][hf]])
    for c in range(NCH):
        pp, bpp = ps_rot.next()
        hf = c // 4
        for j in range(2):
            tr(pp[:, j * 128:(j + 1) * 128], stg_t[j][hf][:, (c % 4) * 128:(c % 4 + 1) * 128], ident,
               [stg_b[j][hf], B_const], [bpp])
        vcopy(memT[:, c, :], pp[:, 0:256], [bpp], [B_memT])

    def layer_norm(t, gcol, bcol, gscol, bscol):
        pm, bpm = ps[4], B_ps[4]
        pe2, bpe2 = ps[5], B_ps[5]
        for c in range(NCH):
            mm(pm[:, :], onesm, xres[:, c, tgs(t)], c == 0, c == NCH - 1, [B_const, B_xres[c][t]], [bpm])
        for c in range(NCH):
            sq, bsq = pt_rot.next()
            act(sq[:, :], xres[:, c, tgs(t)], AF.Square, [B_xres[c][t]], [bsq])
            mm(pe2[:, :], onesm_bf[:, :], sq[:, :], c == 0, c == NCH - 1, [B_tab, bsq], [bpe2])
        mean, bmean = ln_mean, B_lnm
        act(mean[:, :], pm[:, :], AF.Identity, [bpm], [bmean])
        msq, bmsq = tmp_rot.next()
        act(msq[:, :], pm[:, :], AF.Square, [bpm], [bmsq])
        var, bvar = tmp_rot.next()
        vtt(var[:, :], pe2[:, :], msq[:, :], ALU.subtract, [bpe2, bmsq], [bvar])
        act(var[:, :], var[:, :], AF.Ln, [bvar, B_const], [bvar], bias=eps_ap)
        rstd, brstd = ln_rstd, B_lnr
        act(rstd[:, :], var[:, :], AF.Exp, [bvar], [brstd], scale=-0.5)
        for c in range(NCH):
            u, bu = tmp_rot.next()
            vtt(u[:, :], xres[:, c, tgs(t)], mean[:, :], ALU.subtract, [B_xres[c][t], bmean], [bu])
            vtt(u[:, :], u[:, :], rstd[:, :], ALU.mult, [bu, brstd], [bu])
            act(xres[:, c, tgs(t)], u[:, :], AF.Identity, [bu, B_tab], [B_xres[c][t]],
                bias=lnps[:, bscol + c:bscol + c + 1], scale=lnps[:, gscol + c:gscol + c + 1])
            act(xb[:, c, tgs(t)], u[:, :], AF.Identity, [bu, B_const], [B_xb[c][t]],
                bias=spm[:, SP_LN + bcol + c:SP_LN + bcol + c + 1], scale=spm[:, SP_LN + gcol + c:SP_LN + gcol + c + 1])

    acc_rot = Rot([(4, 5), (6, 7)])
    fox_rot = Rot([4, 5, 6, 7])
    s_rot = Rot([0, 1, 2, 3])

    def normalize(po, pd, rows, dst_ap, dst_buf, sink_ap=None):
        tmp, btmp = tmp_rot.next()
        if sink_ap is not None:
            act(tmp[rows, :], ps[pd][rows, :], AF.Ln, [B_ps[pd], B_tab], [btmp], bias=sink_ap)
        else:
            act(tmp[rows, :], ps[pd][rows, :], AF.Ln, [B_ps[pd]], [btmp])
        act(tmp[rows, :], tmp[rows, :], AF.Exp, [btmp], [btmp], scale=-1.0)
        vtt(dst_ap, ps[po][rows, :], tmp[rows, :], ALU.mult, [B_ps[po], btmp], [dst_buf])

    def run_items(items, depth=2):
        n = len(items)
        for i in range(min(depth, n)):
            items[i][0]()
        for i in range(n):
            if i + depth < n:
                items[i + depth][0]()
            items[i][1]()

    class _Stop(Exception):
        pass

    def _chk(tag):
        if _os.environ.get("STOP_AFTER") == tag:
            raise _Stop()

    def _layers():
      for l in range(n_layers):
        _layer(l)

    def _layer(l):
        j = l // 2
        swa = (l % 2 == 0)
        phase_switch()
        wkv, bwkv = wload_in(w_attn_rot, wkv_d[l], [(0, 512)])
        for c in range(2):
            si = s_rot.next()
            for k in range(NCH):
                mm(ps[si][:, 0:256], wkv[:, k, c * 128:(c + 1) * 128], memT[:, k, :], k == 0, k == NCH - 1,
                   [bwkv, B_memT], [B_ps[si]])
            act(kmT[:, c, :], ps[si][:, 0:256], AF.Identity, [B_ps[si]], [B_kmT])
        for mb in range(2):
            si = s_rot.next()
            for k in range(NCH):
                mm(ps[si][:, 0:256], memT[:, k, tbs(mb)], wkv[:, k, 256:512], k == 0, k == NCH - 1,
                   [bwkv, B_memT], [B_ps[si]])
            act(vm[:, mb, :], ps[si][:, 0:256], AF.Identity, [B_ps[si]], [B_vm])

        _chk("A%d" % l)

        def proj_fm(wv, bw, col0, t, si):
            for k in range(NCH):
                mm(ps[si][:, :], wv[:, k, col0:col0 + 128], xb[:, k, tgs(t)], k == 0, k == NCH - 1,
                   [bw, B_xb[k][t]], [B_ps[si]])

        def mem_item(m, t, mb, po, pd):
            c = 6 + m // 2
            r = m % 2
            rows = slice(r * 64, (r + 1) * 64)
            st = {}

            def s1():
                si = s_rot.next()
                mm(ps[si][:, :], kmT[rows, m // 2, tbs(mb)], qa[rows, c, tgs(t)], True, True,
                   [B_kmT, B_qa[c][t]], [B_ps[si]])
                p_, bp_ = pt_rot.next()
                act(p_[:, :], ps[si][:, :], AF.Exp, [B_ps[si]], [bp_], scale=SCALE)
                st["p"] = (p_, bp_)

            def s2():
                p_, bp_ = st["p"]
                mm(ps[po][rows, :], vm[:, mb, m * 64:(m + 1) * 64], p_[:, :], mb == 0, mb == 1,
                   [B_vm, bp_], [B_ps[po]])
                mm(ps[pd][rows, :], ones_bf[:, 0:64], p_[:, :], mb == 0, mb == 1, [B_tab, bp_], [B_ps[pd]])
                if mb == 1:
                    normalize(po, pd, rows, qa[rows, c, tgs(t)], B_qa[c][t])
            return (s1, s2)

        def mem_attention():
            items = []
            for m in range(4):
                for t in range(NTG):
                    po, pd = acc_rot.next()
                    for mb in range(2):
                        items.append(mem_item(m, t, mb, po, pd))
            run_items(items, 2)

        if swa:
            KT2 = aview(KV_OFF, 6144).rearrange("p (g t) -> p g t", g=3)
            B_KT2 = [[Buf(f"kt2_{g}_{t}", B_phase) for t in range(NTG)] for g in range(3)]
            V = aview(KV_OFF + 6144, 3072).rearrange("p (b n) -> p b n", b=NTB)
            B_V = [Buf(f"v{b}", B_phase) for b in range(NTB)]
            wsrc = wswa_d[j]
            for rc in range(9):
                wv, bw = wload_in(w_attn_rot, wsrc, [(rc * 128, 128), (1600 + rc * 128, 128)])
                for t in range(NTG):
                    s1_, s2_ = s_rot.next(), s_rot.next()
                    proj_fm(wv, bw, 0, t, s1_)
                    proj_fm(wv, bw, 128, t, s2_)
                    t1, bt1 = tmp_rot.next()
                    t2, bt2 = tmp_rot.next()
                    vtt(t1[:, :], ps[s1_][:, :], Ctab[:, tgs(t)], ALU.mult, [B_ps[s1_], B_tab], [bt1])
                    vtt(t2[:, :], ps[s2_][:, :], Stab[:, tgs(t)], ALU.mult, [B_ps[s2_], B_tab], [bt2])
                    if rc < 6:
                        vtt(qa[:, rc, tgs(t)], t1[:, :], t2[:, :], ALU.add, [bt1, bt2], [B_qa[rc][t]])
                    else:
                        vtt(KT2[:, rc - 6, tgs(t)], t1[:, :], t2[:, :], ALU.add, [bt1, bt2], [B_KT2[rc - 6][t]])
            wv, bw = wload_in(w_attn_rot, wsrc, [(1152, 448)])
            for b in range(NTB):
                si = s_rot.next()
                for k in range(NCH):
                    mm(ps[si][:, 0:192], xb[:, k, tbs(b)], wv[:, k, 0:192], k == 0, k == NCH - 1,
                       [bw, B_xb[k][b // 4]], [B_ps[si]])
                act(V[:, b, :], ps[si][:, 0:192], AF.Identity, [B_ps[si]], [B_V[b]])
            for c in (6, 7):
                for t in range(NTG):
                    si = s_rot.next()
                    proj_fm(wv, bw, 192 + (c - 6) * 128, t, si)
                    act(qa[:, c, tgs(t)], ps[si][:, :], AF.Identity, [B_ps[si]], [B_qa[c][t]])
            _chk("B%d" % l)
            def swa_item(h, t, qb, po, pd):
                g = h // 4
                c = h // 2
                r = h % 2
                rows = slice(r * 64, (r + 1) * 64)
                lo = 0 if qb > 0 else 128
                qcols = slice((qb % 4) * 128, (qb % 4 + 1) * 128)
                st = {}

                def s1():
                    si = s_rot.next()
                    if qb > 0:
                        mm(ps[si][:, 0:128], KT2[rows, g, tbs(qb - 1)], qa[rows, c, tbs(qb)], True, True,
                           [B_KT2[g][(qb - 1) // 4], B_qa[c][t]], [B_ps[si]])
                    mm(ps[si][:, 128:256], KT2[rows, g, tbs(qb)], qa[rows, c, tbs(qb)], True, True,
                       [B_KT2[g][t], B_qa[c][t]], [B_ps[si]])
                    p_, bp_ = pt_rot.next()
                    act(p_[:, lo:256], ps[si][:, lo:256], AF.Exp, [B_ps[si]], [bp_], scale=SCALE)
                    vtt(p_[:, lo:256], p_[:, lo:256], swam_bf[:, lo:256], ALU.mult, [bp_, B_tab], [bp_])
                    st["p"] = (p_, bp_)

                def s2():
                    p_, bp_ = st["p"]
                    if qb > 0:
                        mm(ps[po][rows, qcols], V[:, qb - 1, g * 64:(g + 1) * 64], p_[:, 0:128], True, False,
                           [B_V[qb - 1], bp_], [B_ps[po]])
                        mm(ps[pd][rows, qcols], ones_bf[:, 0:64], p_[:, 0:128], True, False, [B_tab, bp_], [B_ps[pd]])
                    mm(ps[po][rows, qcols], V[:, qb, g * 64:(g + 1) * 64], p_[:, 128:256], qb == 0, True,
                       [B_V[qb], bp_], [B_ps[po]])
                    mm(ps[pd][rows, qcols], ones_bf[:, 0:64], p_[:, 128:256], qb == 0, True, [B_tab, bp_], [B_ps[pd]])
                    if qb % 4 == 3:
                        normalize(po, pd, rows, qa[rows, c, tgs(t)], B_qa[c][t],
                                  sink_ap=sinkE[rows, j * 12 + h:j * 12 + h + 1])
                return (s1, s2)

            items = []
            for h in range(12):
                for t in range(NTG):
                    po, pd = acc_rot.next()
                    for qb in range(4 * t, 4 * t + 4):
                        items.append(swa_item(h, t, qb, po, pd))
            run_items(items, 3)
            mem_attention()
        else:
            wsrc = wfox_d[j]
            KTP = [aview(KV_OFF, 2048)]
            B_KTP = [[Buf(f"ktp{i}_{t}", B_phase) for t in range(NTG)] for i in range(1)]
            VP = [aview(KV_OFF + 2048, 4096).rearrange("p (b n) -> p b n", b=NTB)]
            B_VP = [[Buf(f"vp{i}_{t}", B_phase) for t in range(NTG)] for i in range(1)]
            pr.add("dve", lambda e, vp0=VP[0]: e.memset(vp0[:, :, 64:192], 1.0), [], B_VP[0])
            Dall = aview(KV_OFF + 6144, 2048)
            B_Dall = [Buf(f"Dall{t}", B_phase) for t in range(NTG)]
            pr.add("dve", lambda e, Dall=Dall: e.memset(Dall[:, :], 0.0), [], B_Dall)
            DTs = [ln_mean, ln_rstd]
            B_DTs = [B_lnm, B_lnr]
            for t in range(NTG):
                si = s_rot.next()
                for k in range(NCH):
                    mm(ps[si][0:12, :], spm[:, SP_WF + j * 96 + k * 12:SP_WF + j * 96 + (k + 1) * 12],
                       xres[:, k, tgs(t)], k == 0, k == NCH - 1, [B_const, B_xres[k][t]], [B_ps[si]])
                e1, be1 = tmp_rot.next()
                act(e1[0:12, :], ps[si][0:12, :], AF.Exp, [B_ps[si], B_tab], [be1],
                    bias=negbf[0:12, j:j + 1], scale=-1.0 / ALPHA)
                act(e1[0:12, :], e1[0:12, :], AF.Ln, [be1], [be1], bias=1.0)
                dt_, bdt_ = DTs[t % 2], B_DTs[t % 2]
                dtp, bdtp = DTs[(t + 1) % 2], B_DTs[(t + 1) % 2]
                if t == 0:
                    pr.add("dve", lambda e, e1=e1, dt_=dt_: e.tensor_tensor_scan(
                        dt_[0:12, :], ones_bf[0:12, :], e1[0:12, :], 0.0, ALU.mult, ALU.subtract),
                        [be1, B_tab], [bdt_])
                else:
                    pr.add("dve", lambda e, e1=e1, dt_=dt_, dtp=dtp: e.tensor_tensor_scan(
                        dt_[0:12, :], ones_bf[0:12, :], e1[0:12, :], dtp[0:12, 511:512],
                        ALU.mult, ALU.subtract), [be1, B_tab, bdtp], [bdt_])
                d8, bd8 = tmp_rot.next()
                vts(d8[0:12, :], dt_[0:12, :], 8.0, None, ALU.mult, None, [bdt_], [bd8])
                vcopy(Dall[0:12, tgs(t)], d8[0:12, :], [bd8], [B_Dall[t]])
                vcopy(Dall[64:76, tgs(t)], d8[0:12, :], [bd8], [B_Dall[t]])
                si2 = s_rot.next()
                for jb in range(4):
                    tr(ps[si2][:, jb * 12:(jb + 1) * 12], dt_[0:12, tbs(jb)], cp[0:12, CP_ID:CP_ID + 12],
                       [bdt_, B_const], [B_ps[si2]])
                act(negD[:, t * 4:(t + 1) * 4, :], ps[si2][:, 0:48].rearrange("p (b h) -> p b h", b=4), AF.Identity,
                    [B_ps[si2]], [B_negD[t]], scale=-1.0)
            wv, bw = wload_in(w_attn_rot, wsrc, [(2304, 256)])
            for c in (6, 7):
                for t in range(NTG):
                    si = s_rot.next()
                    proj_fm(wv, bw, (c - 6) * 128, t, si)
                    act(qa[:, c, tgs(t)], ps[si][:, :], AF.Identity, [B_ps[si]], [B_qa[c][t]])
            for c in range(6):
                slot = 0
                wv, bw = wload_in(w_attn_rot, wsrc, [(c * 128, 128), (768 + c * 128, 128), (1536 + c * 128, 128)])
                ktp = KTP[slot]
                vp = VP[slot]
                for t in range(NTG):
                    si = s_rot.next()
                    proj_fm(wv, bw, 0, t, si)
                    act(qa[:, c, tgs(t)], ps[si][:, :], AF.Identity, [B_ps[si]], [B_qa[c][t]])
                    si = s_rot.next()
                    proj_fm(wv, bw, 128, t, si)
                    vcopy(ktp[:, tgs(t)], ps[si][:, :], [B_ps[si]], [B_KTP[slot][t]])
                    si = s_rot.next()
                    for jb in range(4):
                        b = t * 4 + jb
                        for k in range(NCH):
                            mm(ps[si][:, jb * 128:(jb + 1) * 128], xb[:, k, tbs(b)], wv[:, k, 256:384], k == 0,
                               k == NCH - 1, [bw, B_xb[k][t]], [B_ps[si]])
                    psv = ps[si][:, :].rearrange("p (b n) -> p b n", b=4)
                    act(vp[:, t * 4:(t + 1) * 4, 0:64], psv[:, :, 0:64], AF.Identity, [B_ps[si]], [B_VP[slot][t]])
                    act(vp[:, t * 4:(t + 1) * 4, 192:256], psv[:, :, 64:128], AF.Identity, [B_ps[si]], [B_VP[slot][t]])
                def fox_item(c, t, kb, pos_, ktp, vp):
                    nkb = 4 * t + 4
                    jd = kb - 4 * t
                    c0 = max(jd, 0) * 128
                    last = (kb == nkb - 1)
                    st = {}
                    rws = [slice(0, 64), slice(64, 128)]

                    def s1():
                        sis = [s_rot.next(), s_rot.next()]
                        for r in range(2):
                            mm(ps[sis[r]][:, c0:512], ktp[rws[r], tbs(kb)], qa[rws[r], c, t * 512 + c0:(t + 1) * 512],
                               True, False, [B_KTP[0][kb // 4], B_qa[c][t]], [B_ps[sis[r]]])
                        for r in range(2):
                            h = 2 * c + r
                            mm(ps[sis[r]][:, c0:512], sel_bf[rws[r], h * 128:(h + 1) * 128],
                               Dall[rws[r], t * 512 + c0:(t + 1) * 512], False, True, [B_tab, B_Dall[t]], [B_ps[sis[r]]])
                        pp_ = []
                        for r in range(2):
                            h = 2 * c + r
                            p_, bp_ = pt_rot.next()
                            act(p_[:, c0:512], ps[sis[r]][:, c0:512], AF.Exp, [B_ps[sis[r]], B_negD[kb // 4]], [bp_],
                                bias=negD[:, kb, h:h + 1], scale=SCALE)
                            if jd >= 0:
                                vtt(p_[:, c0:c0 + 128], p_[:, c0:c0 + 128], tri_bf[:, :], ALU.mult, [bp_, B_tab], [bp_])
                            pp_.append((p_, bp_))
                        st["p"] = pp_

                    def s2():
                        for r in range(2):
                            p_, bp_ = st["p"][r]
                            po = pos_[r]
                            mm(ps[po][:, c0:512], vp[:, kb, r * 128:(r + 1) * 128], p_[:, c0:512], kb == 0, last,
                               [B_VP[0][kb // 4], bp_], [B_ps[po]])
                        if last:
                            for r in range(2):
                                po = pos_[r]
                                rows, drows = rws[r], rws[1 - r]
                                tmpn, btmpn = tmp_rot.next()
                                act(tmpn[drows, :], ps[po][drows, :], AF.Ln, [B_ps[po]], [btmpn])
                                act(tmpn[drows, :], tmpn[drows, :], AF.Exp, [btmpn], [btmpn], scale=-1.0)
                                vcopy(tmpn[rows, :], tmpn[drows, :], [btmpn], [btmpn])
                                vtt(qa[rows, c, tgs(t)], ps[po][rows, :], tmpn[rows, :], ALU.mult, [B_ps[po], btmpn],
                                    [B_qa[c][t]])
                    return (s1, s2)

                items = []
                for t in range(NTG):
                    pos_ = (fox_rot.next(), fox_rot.next())
                    for kb in range(4 * t + 4):
                        items.append(fox_item(c, t, kb, pos_, ktp, vp))
                run_items(items, 1)
            mem_attention()

        if ("attn%d" % l) in tap_d:
            td = tap_d["attn%d" % l]
            G_tap = pr.grp()
            for c in range(NCH):
                for t in range(NTG):
                    tt_, btt = tmp_rot.next()
                    vcopy(tt_[:, :], qa[:, c, tgs(t)], [B_qa[c][t]], [btt])
                    dma_sp(td[c * 128:(c + 1) * 128, tgs(t)], tt_[:, :], G_tap, [btt], [B_out])

        _chk("C%d" % l)
        wo_t = [wload_in(w_attn_rot, wout_d[l], [(hh * 512, 512)]) for hh in range(2)]
        gcol, bcol = (l * 4 + 0) * 8, (l * 4 + 1) * 8
        for t in range(NTG):
            for dc in range(NCH):
                wv, bw = wo_t[dc // 4]
                si = s_rot.next()
                for k in range(NCH):
                    mm(ps[si][:, :], wv[:, k, (dc % 4) * 128:(dc % 4 + 1) * 128], qa[:, k, tgs(t)], k == 0, k == NCH - 1,
                       [bw, B_qa[k][t]], [B_ps[si]])
                vtt(xres[:, dc, tgs(t)], xres[:, dc, tgs(t)], ps[si][:, :], ALU.add, [B_xres[dc][t], B_ps[si]],
                    [B_xres[dc][t]])
            layer_norm(t, gcol, bcol, gcol, bcol)

        if ("x1_%d" % l) in tap_d:
            td = tap_d["x1_%d" % l]
            G_tap = pr.grp()
            for c in range(NCH):
                for t in range(NTG):
                    dma_sp(td[c * 128:(c + 1) * 128, tgs(t)], xres[:, c, tgs(t)], G_tap, [B_xres[c][t]], [B_out])

        _chk("D%d" % l)
        phase_switch()

        def ffn_pass(w_in2d, w_out2d, gate):
            for f0 in range(0, NFC, 4):
                fcn = min(4, NFC - f0)
                wa, bwa = wload_in(w_ffn_rot, w_in2d, [(f0 * 128, fcn * 128)])
                wb, bwb = wload_in(w_ffn_rot, w_in2d, [(DFF + f0 * 128, fcn * 128)])
                wo, bwo = wload_rows(w_ffn_rot, w_out2d, f0, fcn, D)
                for t in range(NTG):
                    for fc in range(fcn):
                        sa_i, sb_i = s_rot.next(), s_rot.next()
                        proj_fm(wa, bwa, fc * 128, t, sa_i)
                        proj_fm(wb, bwb, fc * 128, t, sb_i)
                        sa, bsa = tmp_rot.next()
                        act(sa[:, :], ps[sa_i][:, :], AF.Silu, [B_ps[sa_i]], [bsa])
                        if gate is not None:
                            gt_, bgt_ = gate
                            vtt(sa[:, :], sa[:, :], gt_[:, tgs(t)], ALU.mult, [bsa, bgt_[t]], [bsa])
                        vtt(gp[:, fc, tgs(t)], sa[:, :], ps[sb_i][:, :], ALU.mult, [bsa, B_ps[sb_i]], [B_gp[fc][t]])
                for t in range(NTG):
                    for dc in range(NCH):
                        yi = 4 + (dc % 4)
                        for fc in range(fcn):
                            mm(ps[yi][:, :], wo[:, fc, dc * 128:(dc + 1) * 128], gp[:, fc, tgs(t)], fc == 0, fc == fcn - 1,
                               [bwo, B_gp[fc][t]], [B_ps[yi]])
                        vtt(xres[:, dc, tgs(t)], xres[:, dc, tgs(t)], ps[yi][:, :], ALU.add,
                            [B_xres[dc][t], B_ps[yi]], [B_xres[dc][t]])

        if swa:
            ffn_pass(wfi_d[j], wfo_d[j], None)
        else:
            for b in range(NTB):
                si = s_rot.next()
                for k in range(NCH):
                    mm(ps[si][:, 0:8], xres[:, k, tbs(b)], spm[:, SP_WR + j * 64 + k * 8:SP_WR + j * 64 + (k + 1) * 8],
                       k == 0, k == NCH - 1, [B_xres[k][b // 4], B_const], [B_ps[si]])
                sm, bsm = small_rot.next()
                lg = sm[:, 0:8]
                pr.add("dve", lambda e, lg=lg, si=si: e.scalar_tensor_tensor(
                    lg, ps[si][:, 0:8], 1.0 / ALPHA, spm[:, SP_BR + j * 8:SP_BR + (j + 1) * 8], ALU.mult, ALU.add),
                    [B_ps[si], B_const], [bsm])
                sm2, bsm2 = small_rot.next()
                mx = sm2[:, 0:8]
                pr.add("dve", lambda e, mx=mx, lg=lg: e.max(mx, lg), [bsm], [bsm2])
                dd = sm2[:, 8:9]
                vtt(dd, sm2[:, 1:2], sm2[:, 0:1], ALU.subtract, [bsm2], [bsm2])
                ee = sm2[:, 9:10]
                act(ee, dd, AF.Exp, [bsm2], [bsm2])
                ss = sm2[:, 10:11]
                vts(ss, ee, 1.0, None, ALU.add, None, [bsm2], [bsm2])
                g1 = sm2[:, 11:12]
                vrecip(g1, ss, [bsm2], [bsm2])
                g2 = sm2[:, 12:13]
                vtt(g2, ee, g1, ALU.mult, [bsm2], [bsm2])
                m1 = sm[:, 8:16]
                vts(m1, lg, sm2[:, 0:1], g1, ALU.is_equal, ALU.mult, [bsm, bsm2], [bsm])
                sm3, bsm3 = small_rot.next()
                m2 = sm3[:, 0:8]
                vts(m2, lg, sm2[:, 1:2], g2, ALU.is_equal, ALU.mult, [bsm, bsm2], [bsm3])
                vtt(Gt[:, b, :], m1, m2, ALU.add, [bsm, bsm3], [B_Gt[b]])
            if ("gate%d" % l) in tap_d:
                td = tap_d["gate%d" % l]
                G_tap = pr.grp()
                for b in range(NTB):
                    dma_sp(td[tbs(b), :], Gt[:, b, :], G_tap, [B_Gt[b]], [B_out])
            for ex in range(8):
                for t in range(NTG):
                    si = s_rot.next()
                    for jb in range(4):
                        b = t * 4 + jb
                        dg, bdg = tmp_rot.next()
                        vts(dg[:, 0:128], ident, Gt[:, b, ex:ex + 1], None, ALU.mult, None, [B_const, B_Gt[b]], [bdg])
                        mm(ps[si][:, jb * 128:(jb + 1) * 128], cp[:, CP_ONES:CP_ONES + 128],
                           dg[:, 0:128], True, True, [B_const, bdg], [B_ps[si]])
                    act(GB[:, tgs(t)], ps[si][:, :], AF.Identity, [B_ps[si]], [B_GB[t]])
                ffn_pass(wei_d[j, ex], weo_d[j, ex], (GB, B_GB))

        gcol, bcol = (l * 4 + 2) * 8, (l * 4 + 3) * 8
        for t in range(NTG):
            layer_norm(t, gcol, bcol, gcol, bcol)
        if ("x2_%d" % l) in tap_d:
            td = tap_d["x2_%d" % l]
            G_tap = pr.grp()
            for c in range(NCH):
                for t in range(NTG):
                    dma_sp(td[c * 128:(c + 1) * 128, tgs(t)], xres[:, c, tgs(t)], G_tap, [B_xres[c][t]], [B_out])

    try:
        _layers()
    except _Stop:
        pass

    phase_switch()
    ps_rot2 = Rot([(ps[i], B_ps[i]) for i in range(8)])
    k_alt = 0
    for b in range(NTB):
        sj = b % 4
        for half in range(2):
            pp, bpp = ps_rot2.next()
            for cc in range(4):
                c = half * 4 + cc
                tr(pp[:, cc * 128:(cc + 1) * 128], xres[:, c, tbs(b)], ident, [B_xres[c][b // 4], B_const], [bpp])
            if k_alt % 2 == 0:
                act(stg[sj][:, half * 512:(half + 1) * 512], pp[:, :], AF.Identity, [bpp], [B_stg[sj]])
            else:
                vcopy(stg[sj][:, half * 512:(half + 1) * 512], pp[:, :], [bpp], [B_stg[sj]])
            k_alt += 1
        dma_sp(out_d[tbs(b), :], stg[sj], G_stg[sj], [B_stg[sj]], [B_out])
    fin_reads = [B_out] + B_stg
    pr.add("sp", lambda e: None, fin_reads, fin_reads)

    pr.emit(nc, es)
    es.close()
    return nc


_CACHE = {}


def _prep_inputs(inp):
    cols = _swa_cols()
    w_swa = np.ascontiguousarray(inp["w_in_swa"][:, :, cols])
    shared = {
        "w_swa": w_swa,
        "w_fox": np.ascontiguousarray(inp["w_in_fox"]),
        "w_kv": np.ascontiguousarray(inp["w_mem_kv"]),
        "w_out": np.ascontiguousarray(inp["w_out"]),
        "w_ffn_in": np.ascontiguousarray(inp["w_ffn_in"]),
        "w_ffn_out": np.ascontiguousarray(inp["w_ffn_out"]),
        "w_exp_in": np.ascontiguousarray(inp["w_exp_in"]),
        "w_exp_out": np.ascontiguousarray(inp["w_exp_out"]),
        "sp": _make_sp(inp),
        "cp": _make_consts()[0],
        "cx": _make_consts()[1],
    }
    return shared


def kernel(**inputs):
    inp = {k: np.asarray(v) for k, v in inputs.items()}
    n = 8
    if "nc" not in _CACHE:
        _CACHE["nc"] = build(4)
    nc = _CACHE["nc"]
    shared = _prep_inputs(inp)
    in_maps = []
    for b in range(n):
        m = dict(shared)
        m["x"] = np.ascontiguousarray(inp["x"][b])
        m["mem"] = np.ascontiguousarray(inp["mem"][b])
        m["pos"] = np.ascontiguousarray(inp["positions"][b].reshape(1, S).astype(np.int32))
        in_maps.append(m)
    res = run_bass_kernel_spmd(nc, in_maps, core_ids=list(range(n)))
    return np.stack([np.asarray(r["out"]) for r in res.results], axis=0).astype(np.float32)
```

```python
import numpy as np
from contextlib import ExitStack
import concourse.bass as bass
import concourse.mybir as mybir
from concourse.bass_utils import run_bass_kernel_spmd

F32 = mybir.dt.float32
BF16 = mybir.dt.bfloat16
I32 = mybir.dt.int32
AF = mybir.ActivationFunctionType
ALU = mybir.AluOpType

S = 2048
D = 1024
NCH = 8
NTG = 4
NTB = 16
DFF = 2816
NFC = 22
ALPHA = 8.0 ** 0.25
SCALE = 0.125
EPOCH = 12000
PI = float(np.pi)


class Buf:
    __slots__ = ("name", "lw", "rd", "phase")

    def __init__(self, name, phase=None):
        self.name = name
        self.lw = None
        self.rd = []
        self.phase = phase


class SemGrp:
    __slots__ = ("sem", "cnt", "batch", "idx")

    def __init__(self, batch=False):
        self.sem = None
        self.cnt = 0
        self.batch = batch


class Op:
    __slots__ = ("eng", "fn", "deps", "sig", "grp", "count", "dma", "epoch")


class Prog:
    def __init__(self):
        self.ops = []
        self.grps = []

    def grp(self, batch=False):
        g = SemGrp(batch)
        self.grps.append(g)
        return g

    def add(self, eng, fn, reads=(), writes=(), dma=None):
        op = Op()
        op.eng = eng
        op.fn = fn
        op.dma = dma is not None
        op.grp = dma
        op.sig = op.dma
        op.count = 0
        op.epoch = 0
        reads = list(reads)
        writes = list(writes)
        for b in list(reads) + list(writes):
            if b.phase is not None:
                reads.append(b.phase)
        deps = {}
        for b in reads:
            if b.lw is not None:
                deps[id(b.lw)] = (b.lw, True)
        for b in writes:
            if b.lw is not None and id(b.lw) not in deps:
                deps[id(b.lw)] = (b.lw, False)
            for r in b.rd:
                if id(r) not in deps:
                    deps[id(r)] = (r, False)
        fin = []
        for p, raw in deps.values():
            if p is op:
                continue
            if op.dma and p.dma and p.grp is op.grp and op.grp.batch:
                continue
            if (not op.dma) and (not p.dma) and p.eng == eng:
                if eng == "pe":
                    continue
            fin.append(p)
            p.sig = True
        op.deps = fin
        for b in reads:
            if not op.dma:
                b.rd = [r for r in b.rd if r.dma or r.eng != eng]
            b.rd.append(op)
        for b in writes:
            b.lw = op
            b.rd = []
        self.ops.append(op)
        return op

    def emit(self, nc, es):
        engs = ["pe", "act", "dve", "pool", "sp"]
        ecount = {e: 0 for e in engs}
        eepoch = {e: 0 for e in engs}
        for op in self.ops:
            if op.dma:
                op.grp.cnt += 16
                op.count = op.grp.cnt
            elif op.sig:
                if ecount[op.eng] >= EPOCH:
                    ecount[op.eng] = 0
                    eepoch[op.eng] += 1
                ecount[op.eng] += 1
                op.count = ecount[op.eng]
                op.epoch = eepoch[op.eng]
        for gi, g in enumerate(self.grps):
            if g.cnt > 0:
                g.sem = es.enter_context(nc.semaphore(f"sg{gi}"))
        esem = {}
        for e in engs:
            for k in range(eepoch[e] + 1):
                esem[(e, k)] = es.enter_context(nc.semaphore(f"se_{e}_{k}"))
        for op in self.ops:
            if op.dma and op.grp.batch:
                op.count = op.grp.cnt
        block = es.enter_context(nc.Block())
        streams = {e: [o for o in self.ops if o.eng == e] for e in engs}

        def run(engname, eobj):
            waited = {}
            for op in streams[engname]:
                need = {}
                for p in op.deps:
                    if p.dma:
                        key = ("g", id(p.grp))
                        sem = p.grp.sem
                    else:
                        key = (p.eng, p.epoch)
                        sem = esem[key]
                    if waited.get(key, 0) >= p.count:
                        continue
                    if key not in need or need[key][1] < p.count:
                        need[key] = (sem, p.count)
                for key, (sem, cnt) in need.items():
                    eobj.wait_ge(sem, cnt)
                    waited[key] = cnt
                ins = op.fn(eobj)
                if ins is None:
                    continue
                if op.dma:
                    ins.then_inc(op.grp.sem, 16)
                elif op.sig:
                    ins.then_inc(esem[(op.eng, op.epoch)], 1)

        if streams["pe"]:
            @block.tensor
            def _(e):
                run("pe", e)

        if streams["act"]:
            @block.scalar
            def _(e):
                run("act", e)

        if streams["dve"]:
            @block.vector
            def _(e):
                run("dve", e)

        if streams["pool"]:
            @block.gpsimd
            def _(e):
                run("pool", e)

        if streams["sp"]:
            @block.sync
            def _(e):
                run("sp", e)


class Rot:
    def __init__(self, items):
        self.items = items
        self.i = 0

    def next(self):
        it = self.items[self.i % len(self.items)]
        self.i += 1
        return it


NCP = 128 + 128 + 128 + 3
CP_ID, CP_ONESM, CP_ONES, CP_INVF = 0, 128, 256, 384
CP_SIGN, CP_EPS = CP_INVF + 1, CP_INVF + 2
NCX = 128 + 256 + 12 * 128 + 512
CX_TRI, CX_SWAM, CX_SEL, CX_ONES = 0, 128, 384, 384 + 1536
NSP = 128 + 24 + 16 + 2 + 128 + 192
SP_LN, SP_SINK, SP_BR, SP_BF, SP_WR, SP_WF = 0, 128, 152, 168, 170, 298
SWA_EXT = 2752


def _swap16(d):
    return d + 8 if d < 8 else (d - 8 if d < 16 else d)


def _make_consts():
    cp = np.zeros((128, NCP), np.float32)
    cx = np.zeros((128, NCX), np.float32)
    cp[:, CP_ID:CP_ID + 128] = np.eye(128, dtype=np.float32)
    cp[:, CP_ONESM:CP_ONESM + 128] = 1.0 / 1024.0
    k = np.arange(128)[:, None]
    q = np.arange(128)[None, :]
    cx[:, CX_TRI:CX_TRI + 128] = (q >= k).astype(np.float32)
    mp = np.ones((128, 128), np.float32)
    mp[:64, 64:] = 0.0
    mo = np.ones((128, 128), np.float32)
    mo[64:, :64] = 0.0
    cx[:, CX_SWAM:CX_SWAM + 128] = mp
    cx[:, CX_SWAM + 128:CX_SWAM + 256] = mo
    cp[:, CP_ONES:CP_ONES + 128] = 1.0
    cx[:, CX_ONES:CX_ONES + 512] = 1.0
    for h in range(12):
        cx[h, CX_SEL + h * 128:CX_SEL + (h + 1) * 128] = 1.0
        cx[64 + h, CX_SEL + h * 128:CX_SEL + (h + 1) * 128] = 1.0
    inv_freq = (500000.0 ** (-np.arange(0, 16, 2, dtype=np.float32) / 16)).astype(np.float32)
    for p in range(128):
        d = p % 64
        if d < 16:
            cp[p, CP_INVF] = inv_freq[d % 8]
            cp[p, CP_SIGN] = -1.0 if d < 8 else 1.0
    cp[:, CP_EPS] = 1e-5
    return cp, cx


def _make_sp(inp):
    sp = np.zeros((128, NSP), np.float32)
    vecs = [inp["ln_attn_g"], inp["ln_attn_b"], inp["ln_ffn_g"], inp["ln_ffn_b"]]
    for l in range(4):
        for j in range(4):
            sp[:, SP_LN + (l * 4 + j) * 8:SP_LN + (l * 4 + j) * 8 + 8] = vecs[j][l].reshape(8, 128).T
    sp[:, SP_SINK:SP_SINK + 24] = inp["attn_sinks"].reshape(1, 24)
    sp[:, SP_BR:SP_BR + 16] = inp["b_router"].reshape(1, 16)
    sp[:12, SP_BF:SP_BF + 2] = inp["b_forget"].T
    for j in range(2):
        sp[:, SP_WR + j * 64:SP_WR + (j + 1) * 64] = (
            inp["w_router"][j].reshape(8, 128, 8).transpose(1, 0, 2).reshape(128, 64))
        sp[:, SP_WF + j * 96:SP_WF + (j + 1) * 96] = (
            inp["w_in_fox"][j][:, 2560:2572].reshape(8, 128, 12).transpose(1, 0, 2).reshape(128, 96))
    return sp


def _swa_cols():
    main = list(range(768))
    for g in range(3):
        for r in range(2):
            main += [768 + g * 64 + d for d in range(64)]
    main += list(range(960, 1408))
    sw = []
    for h in range(12):
        sw += [h * 64 + _swap16(d) for d in range(64)]
    for g in range(3):
        for r in range(2):
            sw += [768 + g * 64 + _swap16(d) for d in range(64)]
    return np.array(main + sw, np.int64)


def build(n_layers=4, taps=()):
    import os as _os
    nc = bass.Bass("TRN2", target_bir_lowering=False)
    pr = Prog()
    es = ExitStack()

    def din(name, shape, dt=F32):
        return nc.dram_tensor(name, list(shape), dt, kind="ExternalInput").ap()

    x_d = din("x", [S, D])
    mem_d = din("mem", [256, D])
    pos_d = din("pos", [1, S], I32)
    if n_layers >= 1:
        wswa_d = din("w_swa", [2, D, SWA_EXT])
        wkv_d = din("w_kv", [4, D, 512])
        wout_d = din("w_out", [4, D, D])
        wfi_d = din("w_ffn_in", [2, D, 2 * DFF])
        wfo_d = din("w_ffn_out", [2, DFF, D])
    if n_layers >= 2:
        wfox_d = din("w_fox", [2, D, 2572])
        wei_d = din("w_exp_in", [2, 8, D, 2 * DFF])
        weo_d = din("w_exp_out", [2, 8, DFF, D])
    sp_d = din("sp", [128, NSP])
    cp_d = din("cp", [128, NCP])
    cx_d = din("cx", [128, NCX])
    out_d = nc.dram_tensor("out", [S, D], F32, kind="ExternalOutput").ap()
    tap_d = {}
    for name, shape in taps:
        tap_d[name] = nc.dram_tensor(name, list(shape), F32, kind="ExternalOutput").ap()

    def sb(name, shape, dt):
        return es.enter_context(nc.sbuf_tensor("s_" + name, list(shape), dt))

    xres = sb("xres", [128, NCH, S], F32)
    xb = sb("xb", [128, NCH, S], BF16)
    arena = sb("arena", [128, 25600], BF16)
    wbase = [sb(f"wbase{i}", [128, 4096], BF16) for i in range(2)]
    Ctab = sb("Ctab", [128, S], BF16)
    Stab = sb("Stab", [128, S], BF16)
    cp = sb("cp", [128, NCP], F32)
    spm = sb("spm", [128, NSP], F32)
    lnps = sb("lnps", [128, 128], F32)
    sinkE = sb("sinkE", [128, 24], F32)
    negbf = sb("negbf", [128, 2], F32)
    tri_bf = sb("tri_bf", [128, 128], BF16)
    swam_bf = sb("swam_bf", [128, 256], BF16)
    ones_bf = sb("ones_bf", [128, 512], BF16)
    sel_bf = sb("sel_bf", [128, 1536], BF16)
    memT = sb("memT", [128, NCH, 256], BF16)
    kmT = sb("kmT", [128, 2, 256], BF16)
    vm = sb("vm", [128, 2, 256], BF16)
    negD = sb("negD", [128, NTB, 12], F32)
    Gt = sb("Gt", [128, NTB, 8], F32)
    tmpf = [sb(f"tmpf{i}", [128, 512], F32) for i in range(6)]
    onesm_bf = sb("onesm_bf", [128, 128], BF16)
    ln_mean = sb("ln_mean", [128, 512], F32)
    ln_rstd = sb("ln_rstd", [128, 512], F32)
    B_lnm = Buf("ln_mean")
    B_lnr = Buf("ln_rstd")
    ptb = [sb(f"pt{i}", [128, 512], BF16) for i in range(4)]
    smallf = [sb(f"small{i}", [128, 16], F32) for i in range(8)]
    ps = [es.enter_context(nc.psum_tensor(f"p_ps{i}", [128, 512], F32)) for i in range(8)]

    B_xres = [[Buf(f"xres{c}_{t}") for t in range(NTG)] for c in range(NCH)]
    B_xb = [[Buf(f"xb{c}_{t}") for t in range(NTG)] for c in range(NCH)]
    B_phase = Buf("phase")
    B_ps = [Buf(f"ps{i}") for i in range(8)]
    B_tmpf = [Buf(f"tmpf{i}") for i in range(6)]
    B_pt = [Buf(f"pt{i}") for i in range(4)]
    B_small = [Buf(f"small{i}") for i in range(8)]
    B_wbase = [Buf(f"wbase{i}") for i in range(2)]
    G_wbase = [pr.grp() for _ in range(2)]
    B_const = Buf("const")
    B_tab = Buf("tab")
    B_memT = Buf("memT")
    B_kmT = Buf("kmT")
    B_vm = Buf("vm")
    B_negD = [Buf(f"negD{t}") for t in range(NTG)]
    B_Gt = [Buf(f"Gt{t}") for t in range(NTB)]
    B_out = Buf("outdram")
    B_outs = [Buf(f"outdram{b}") for b in range(NTB)]
    G_const = pr.grp(batch=True)

    tmp_rot = Rot(list(zip(tmpf, B_tmpf)))
    pt_rot = Rot(list(zip(ptb, B_pt)))
    small_rot = Rot(list(zip(smallf, B_small)))

    def tgs(t):
        return slice(t * 512, (t + 1) * 512)

    def tbs(t):
        return slice(t * 128, (t + 1) * 128)

    def mm(out, lhsT, rhs, start, stop, reads, writes):
        pr.add("pe", lambda e: e.matmul(out, lhsT, rhs, start=start, stop=stop), reads, writes)

    def tr(out, in_, ident, reads, writes):
        pr.add("pe", lambda e: e.transpose(out, in_, ident), reads, writes)

    def act(out, in_, func, reads, writes, bias=None, scale=None):
        kw = {}
        if bias is not None:
            kw["bias"] = bias
        if scale is not None:
            kw["scale"] = scale
        pr.add("act", lambda e: e.activation(out, in_, func, **kw), reads, writes)

    def vtt(out, a, b, op, reads, writes):
        pr.add("dve", lambda e: e.tensor_tensor(out, a, b, op), reads, writes)

    def vts(out, a, s1, s2, op0, op1, reads, writes):
        if op1 is None:
            pr.add("dve", lambda e: e.tensor_scalar(out, a, s1, None, op0), reads, writes)
        else:
            pr.add("dve", lambda e: e.tensor_scalar(out, a, s1, s2, op0, op1), reads, writes)

    def vcopy(out, a, reads, writes):
        pr.add("dve", lambda e: e.tensor_copy(out, a), reads, writes)

    def vrecip(out, a, reads, writes):
        pr.add("dve", lambda e: e.reciprocal(out, a), reads, writes)

    def dma_sp(out, in_, grp, reads, writes):
        pr.add("sp", lambda e: e.dma_start(out=out, in_=in_), reads, writes, dma=grp)

    def dma_pool(out, in_, grp, reads, writes):
        pr.add("pool", lambda e: e.dma_start(out=out, in_=in_), reads, writes, dma=grp)

    def phase_switch():
        sm, bsm = small_rot.next()
        pr.add("dve", lambda e: e.memset(sm[:, 0:1], 0.0), [], [bsm, B_phase])

    def aview(off, n):
        return arena[:, off:off + n]

    QA_OFF = 0
    KV_OFF = 16384
    qa = aview(QA_OFF, 16384).rearrange("p (c t) -> p c t", c=NCH)
    B_qa = [[Buf(f"qa{c}_{t}", B_phase) for t in range(NTG)] for c in range(NCH)]
    gp = aview(0, 8192).rearrange("p (c t) -> p c t", c=4)
    B_gp = [[Buf(f"gp{c}_{t}", B_phase) for t in range(NTG)] for c in range(4)]
    GB = aview(8192, 4096).bitcast(F32)
    B_GB = [Buf(f"GB{t}", B_phase) for t in range(NTG)]
    wext = [aview(12288 + i * 4096, 4096) for i in range(3)]
    B_wext = [Buf(f"wext{i}", B_phase) for i in range(3)]
    G_wext = [pr.grp() for _ in range(3)]
    stg = [aview(i * 2048, 2048).bitcast(F32) for i in range(4)]
    if _os.environ.get('DED_STG'):
        stg = [tmpf[i][:, :] for i in range(2)] * 2
        stg = [sb(f"stgd{i}", [128, 1024], F32)[:, :] for i in range(4)]
    B_stg = [Buf(f"stg{i}", B_phase) for i in range(4)]
    G_stg = [pr.grp() for _ in range(4)]

    w_attn_rot = Rot([(wbase[i][:, :], B_wbase[i], G_wbase[i]) for i in range(2)])
    w_ffn_rot = Rot([(wbase[i][:, :], B_wbase[i], G_wbase[i]) for i in range(2)]
                    + [(wext[i], B_wext[i], G_wext[i]) for i in range(3)])

    def wload_in(rot, src2d, col_list):
        wt, bw, gw = rot.next()
        ntot = sum(n for _, n in col_list)
        v = wt[:, 0:8 * ntot].rearrange("p (k n) -> p k n", k=8)
        o = 0
        for c0, n in col_list:
            dma_pool(v[:, :, o:o + n], src2d[:, c0:c0 + n].rearrange("(k p) n -> p k n", p=128), gw, [], [bw])
            o += n
        return v, bw

    def wload_rows(rot, src2d, r0, nchunks, ncols):
        wt, bw, gw = rot.next()
        v = wt[:, 0:nchunks * ncols].rearrange("p (k n) -> p k n", k=nchunks)
        dma_pool(v, src2d[r0 * 128:(r0 + nchunks) * 128, :].rearrange("(k p) n -> p k n", p=128), gw, [], [bw])
        return v, bw

    ident = cp[:, CP_ID:CP_ID + 128]
    onesm = cp[:, CP_ONESM:CP_ONESM + 128]
    eps_ap = cp[:, CP_EPS:CP_EPS + 1]

    import os as _os
    dma_sp(cp[:, :], cp_d, G_const, [], [B_const])
    dma_sp(spm[:, :], sp_d, G_const, [], [B_const])
    if _os.environ.get('SKIP_CX'):
        cx_d = None
    cxs = aview(8192, 2 * NCX).bitcast(F32)
    B_cxs = Buf("cxs", B_phase)
    G_cxs = pr.grp()
    if cx_d is not None:
        dma_sp(cxs, cx_d, G_cxs, [], [B_cxs])
        vcopy(tri_bf[:, :], cxs[:, CX_TRI:CX_TRI + 128], [B_cxs], [B_tab])
        vcopy(swam_bf[:, :], cxs[:, CX_SWAM:CX_SWAM + 256], [B_cxs], [B_tab])
        vcopy(ones_bf[:, :], cxs[:, CX_ONES:CX_ONES + 512], [B_cxs], [B_tab])
        vcopy(sel_bf[:, :], cxs[:, CX_SEL:CX_SEL + 1536], [B_cxs], [B_tab])
    vts(lnps[:, :], spm[:, SP_LN:SP_LN + 128], ALPHA, None, ALU.mult, None, [B_const], [B_tab])
    lastl = n_layers - 1
    if lastl >= 0:
        c0 = (lastl * 4 + 2) * 8
        vcopy(lnps[:, c0:c0 + 16], spm[:, SP_LN + c0:SP_LN + c0 + 16], [B_const, B_tab], [B_tab])
    if not _os.environ.get("SKIP_SINK"):
        act(sinkE[:, :], spm[:, SP_SINK:SP_SINK + 24], AF.Exp, [B_const], [B_tab])
    vts(negbf[:, :], spm[:, SP_BF:SP_BF + 2], -1.0, None, ALU.mult, None, [B_const], [B_tab])
    vcopy(onesm_bf[:, :], cp[:, CP_ONESM:CP_ONESM + 128], [B_const], [B_tab])

    import os as _os
    G_pos = pr.grp()
    for t in range(NTG if not _os.environ.get("SKIP_ROPE") else 0):
        pt_, bpt = tmpf[0], B_tmpf[0]
        pi_ = pt_[:, :].bitcast(I32)
        dma_sp(pi_, pos_d[0:1, tgs(t)].partition_broadcast(128), G_pos, [], [bpt])
        pf, bpf = tmpf[1], B_tmpf[1]
        vcopy(pf[:, :], pi_, [bpt], [bpf])
        ang, bang = tmpf[2], B_tmpf[2]
        vts(ang[:, :], pf[:, :], cp[:, CP_INVF:CP_INVF + 1], None, ALU.mult, None, [bpf, B_const], [bang])
        for which in range(2):
            a2, ba2 = tmpf[3], B_tmpf[3]
            if which == 1:
                vts(a2[:, :], ang[:, :], PI / 2, None, ALU.add, None, [bang], [ba2])
            else:
                vcopy(a2[:, :], ang[:, :], [bang], [ba2])
            u, bu = tmpf[4], B_tmpf[4]
            vts(u[:, :], a2[:, :], 1.0 / (2 * PI), 0.5, ALU.mult, ALU.add, [ba2], [bu])
            ui = pt_[:, :].bitcast(I32)
            vcopy(ui, u[:, :], [bu], [bpt])
            vcopy(u[:, :], ui, [bpt], [bu])
            C1, C2, C3 = 6.28125, 0.0019350051879882812, 3.019916050561005e-07
            for cc in (C1, C2, C3):
                pr.add("dve", (lambda cc_: lambda e: e.scalar_tensor_tensor(a2[:, :], u[:, :], -cc_, a2[:, :], ALU.mult, ALU.add))(cc),
                       [bu, ba2], [ba2])
            m, bm = tmpf[5], B_tmpf[5]
            vts(m[:, :], a2[:, :], PI, -2 * PI, ALU.is_gt, ALU.mult, [ba2], [bm])
            vtt(a2[:, :], a2[:, :], m[:, :], ALU.add, [ba2, bm], [ba2])
            vts(m[:, :], a2[:, :], -PI, 2 * PI, ALU.is_lt, ALU.mult, [ba2], [bm])
            vtt(a2[:, :], a2[:, :], m[:, :], ALU.add, [ba2, bm], [ba2])
            vts(a2[:, :], a2[:, :], PI, -PI, ALU.min, ALU.max, [ba2], [ba2])
            if which == 1:
                act(Ctab[:, tgs(t)], a2[:, :], AF.Sin, [ba2], [B_tab])
            else:
                act(a2[:, :], a2[:, :], AF.Sin, [ba2], [ba2])
                vts(Stab[:, tgs(t)], a2[:, :], cp[:, CP_SIGN:CP_SIGN + 1], None, ALU.mult, None, [ba2, B_const], [B_tab])

    ps_rot = Rot([(ps[i], B_ps[i]) for i in range(4)])
    stg_t = [(tmpf[0], tmpf[1]), (tmpf[2], tmpf[3]), (tmpf[4], tmpf[5]), (ln_mean, ln_rstd)]
    stg_b = [(B_tmpf[0], B_tmpf[1]), (B_tmpf[2], B_tmpf[3]), (B_tmpf[4], B_tmpf[5]), (B_lnm, B_lnr)]
    G_st2 = [[pr.grp() for _ in range(2)] for _ in range(4)]
    for t in range(NTG):
        for j in range(4):
            for hf in range(2):
                dma_sp(stg_t[j][hf][:, :], x_d[tbs(t * 4 + j), hf * 512:(hf + 1) * 512], G_st2[j][hf], [], [stg_b[j][hf]])
        for c in range(NCH):
            pp, bpp = ps_rot.next()
            hf = c // 4
            for j in range(4):
                tr(pp[:, j * 128:(j + 1) * 128], stg_t[j][hf][:, (c % 4) * 128:(c % 4 + 1) * 128], ident,
                   [stg_b[j][hf], B_const], [bpp])
            act(xres[:, c, tgs(t)], pp[:, :], AF.Identity, [bpp], [B_xres[c][t]], scale=ALPHA)
            act(xb[:, c, tgs(t)], pp[:, :], AF.Identity, [bpp], [B_xb[c][t]])
    for j in range(2):
        for hf in range(2):
            dma_sp(stg_t[j][hf][:, :], mem_d[tbs(j), hf * 512:(hf + 1) * 512], G_st2[j][hf], [], [stg_b[j][hf]])
    for c in range(NCH):
        pp, bpp = ps_rot.next()
        hf = c // 4
        for j in range(2):
            tr(pp[:, j * 128:(j + 1) * 128], stg_t[j][hf][:, (c % 4) * 128:(c % 4 + 1) * 128], ident,
               [stg_b[j][hf], B_const], [bpp])
        vcopy(memT[:, c, :], pp[:, 0:256], [bpp], [B_memT])

    def layer_norm(t, gcol, bcol, gscol, bscol):
        pm, bpm = ps[4], B_ps[4]
        pe2, bpe2 = ps[5], B_ps[5]
        for c in range(NCH):
            mm(pm[:, :], onesm, xres[:, c, tgs(t)], c == 0, c == NCH - 1, [B_const, B_xres[c][t]], [bpm])
        for c in range(NCH):
            sq, bsq = pt_rot.next()
            act(sq[:, :], xres[:, c, tgs(t)], AF.Square, [B_xres[c][t]], [bsq])
            mm(pe2[:, :], onesm_bf[:, :], sq[:, :], c == 0, c == NCH - 1, [B_tab, bsq], [bpe2])
        mean, bmean = ln_mean, B_lnm
        act(mean[:, :], pm[:, :], AF.Identity, [bpm], [bmean])
        msq, bmsq = tmp_rot.next()
        act(msq[:, :], pm[:, :], AF.Square, [bpm], [bmsq])
        var, bvar = tmp_rot.next()
        vtt(var[:, :], pe2[:, :], msq[:, :], ALU.subtract, [bpe2, bmsq], [bvar])
        act(var[:, :], var[:, :], AF.Ln, [bvar, B_const], [bvar], bias=eps_ap)
        rstd, brstd = ln_rstd, B_lnr
        act(rstd[:, :], var[:, :], AF.Exp, [bvar], [brstd], scale=-0.5)
        for c in range(NCH):
            u, bu = tmp_rot.next()
            vtt(u[:, :], xres[:, c, tgs(t)], mean[:, :], ALU.subtract, [B_xres[c][t], bmean], [bu])
            vtt(u[:, :], u[:, :], rstd[:, :], ALU.mult, [bu, brstd], [bu])
            act(xres[:, c, tgs(t)], u[:, :], AF.Identity, [bu, B_tab], [B_xres[c][t]],
                bias=lnps[:, bscol + c:bscol + c + 1], scale=lnps[:, gscol + c:gscol + c + 1])
            act(xb[:, c, tgs(t)], u[:, :], AF.Identity, [bu, B_const], [B_xb[c][t]],
                bias=spm[:, SP_LN + bcol + c:SP_LN + bcol + c + 1], scale=spm[:, SP_LN + gcol + c:SP_LN + gcol + c + 1])

    acc_rot = Rot([(4, 5), (6, 7)])
    fox_rot = Rot([4, 5, 6, 7])
    s_rot = Rot([0, 1, 2, 3])

    def normalize(po, pd, rows, dst_ap, dst_buf, sink_ap=None):
        tmp, btmp = tmp_rot.next()
        if sink_ap is not None:
            act(tmp[rows, :], ps[pd][rows, :], AF.Ln, [B_ps[pd], B_tab], [btmp], bias=sink_ap)
        else:
            act(tmp[rows, :], ps[pd][rows, :], AF.Ln, [B_ps[pd]], [btmp])
        act(tmp[rows, :], tmp[rows, :], AF.Exp, [btmp], [btmp], scale=-1.0)
        vtt(dst_ap, ps[po][rows, :], tmp[rows, :], ALU.mult, [B_ps[po], btmp], [dst_buf])

    def run_items(items, depth=2):
        n = len(items)
        for i in range(min(depth, n)):
            items[i][0]()
        for i in range(n):
            if i + depth < n:
                items[i + depth][0]()
            items[i][1]()

    class _Stop(Exception):
        pass

    def _chk(tag):
        if _os.environ.get("STOP_AFTER") == tag:
            raise _Stop()

    def _layers():
      for l in range(n_layers):
        _layer(l)

    def _layer(l):
        j = l // 2
        swa = (l % 2 == 0)
        phase_switch()
        wkv, bwkv = wload_in(w_attn_rot, wkv_d[l], [(0, 512)])
        for c in range(2):
            si = s_rot.next()
            for k in range(NCH):
                mm(ps[si][:, 0:256], wkv[:, k, c * 128:(c + 1) * 128], memT[:, k, :], k == 0, k == NCH - 1,
                   [bwkv, B_memT], [B_ps[si]])
            act(kmT[:, c, :], ps[si][:, 0:256], AF.Identity, [B_ps[si]], [B_kmT])
        for mb in range(2):
            si = s_rot.next()
            for k in range(NCH):
                mm(ps[si][:, 0:256], memT[:, k, tbs(mb)], wkv[:, k, 256:512], k == 0, k == NCH - 1,
                   [bwkv, B_memT], [B_ps[si]])
            act(vm[:, mb, :], ps[si][:, 0:256], AF.Identity, [B_ps[si]], [B_vm])

        _chk("A%d" % l)

        def proj_fm(wv, bw, col0, t, si):
            for k in range(NCH):
                mm(ps[si][:, :], wv[:, k, col0:col0 + 128], xb[:, k, tgs(t)], k == 0, k == NCH - 1,
                   [bw, B_xb[k][t]], [B_ps[si]])

        def mem_item(m, t, mb, po, pd):
            c = 6 + m // 2
            r = m % 2
            rows = slice(r * 64, (r + 1) * 64)
            st = {}

            def s1():
                si = s_rot.next()
                mm(ps[si][:, :], kmT[rows, m // 2, tbs(mb)], qa[rows, c, tgs(t)], True, True,
                   [B_kmT, B_qa[c][t]], [B_ps[si]])
                p_, bp_ = pt_rot.next()
                act(p_[:, :], ps[si][:, :], AF.Exp, [B_ps[si]], [bp_], scale=SCALE)
                st["p"] = (p_, bp_)

            def s2():
                p_, bp_ = st["p"]
                mm(ps[po][rows, :], vm[:, mb, m * 64:(m + 1) * 64], p_[:, :], mb == 0, mb == 1,
                   [B_vm, bp_], [B_ps[po]])
                mm(ps[pd][rows, :], ones_bf[:, 0:64], p_[:, :], mb == 0, mb == 1, [B_tab, bp_], [B_ps[pd]])
                if mb == 1:
                    normalize(po, pd, rows, qa[rows, c, tgs(t)], B_qa[c][t])
            return (s1, s2)

        def mem_attention():
            items = []
            for m in range(4):
                for t in range(NTG):
                    po, pd = acc_rot.next()
                    for mb in range(2):
                        items.append(mem_item(m, t, mb, po, pd))
            run_items(items, 2)

        if swa:
            KT2 = aview(KV_OFF, 6144).rearrange("p (g t) -> p g t", g=3)
            B_KT2 = [[Buf(f"kt2_{g}_{t}", B_phase) for t in range(NTG)] for g in range(3)]
            V = aview(KV_OFF + 6144, 3072).rearrange("p (b n) -> p b n", b=NTB)
            B_V = [Buf(f"v{b}", B_phase) for b in range(NTB)]
            wsrc = wswa_d[j]
            for rc in range(9):
                wv, bw = wload_in(w_attn_rot, wsrc, [(rc * 128, 128), (1600 + rc * 128, 128)])
                for t in range(NTG):
                    s1_, s2_ = s_rot.next(), s_rot.next()
                    proj_fm(wv, bw, 0, t, s1_)
                    proj_fm(wv, bw, 128, t, s2_)
                    t1, bt1 = tmp_rot.next()
                    t2, bt2 = tmp_rot.next()
                    vtt(t1[:, :], ps[s1_][:, :], Ctab[:, tgs(t)], ALU.mult, [B_ps[s1_], B_tab], [bt1])
                    vtt(t2[:, :], ps[s2_][:, :], Stab[:, tgs(t)], ALU.mult, [B_ps[s2_], B_tab], [bt2])
                    if rc < 6:
                        vtt(qa[:, rc, tgs(t)], t1[:, :], t2[:, :], ALU.add, [bt1, bt2], [B_qa[rc][t]])
                    else:
                        vtt(KT2[:, rc - 6, tgs(t)], t1[:, :], t2[:, :], ALU.add, [bt1, bt2], [B_KT2[rc - 6][t]])
            wv, bw = wload_in(w_attn_rot, wsrc, [(1152, 448)])
            for b in range(NTB):
                si = s_rot.next()
                for k in range(NCH):
                    mm(ps[si][:, 0:192], xb[:, k, tbs(b)], wv[:, k, 0:192], k == 0, k == NCH - 1,
                       [bw, B_xb[k][b // 4]], [B_ps[si]])
                act(V[:, b, :], ps[si][:, 0:192], AF.Identity, [B_ps[si]], [B_V[b]])
            for c in (6, 7):
                for t in range(NTG):
                    si = s_rot.next()
                    proj_fm(wv, bw, 192 + (c - 6) * 128, t, si)
                    act(qa[:, c, tgs(t)], ps[si][:, :], AF.Identity, [B_ps[si]], [B_qa[c][t]])
            _chk("B%d" % l)
            def swa_item(h, t, qb, po, pd):
                g = h // 4
                c = h // 2
                r = h % 2
                rows = slice(r * 64, (r + 1) * 64)
                lo = 0 if qb > 0 else 128
                qcols = slice((qb % 4) * 128, (qb % 4 + 1) * 128)
                st = {}

                def s1():
                    si = s_rot.next()
                    if qb > 0:
                        mm(ps[si][:, 0:128], KT2[rows, g, tbs(qb - 1)], qa[rows, c, tbs(qb)], True, True,
                           [B_KT2[g][(qb - 1) // 4], B_qa[c][t]], [B_ps[si]])
                    mm(ps[si][:, 128:256], KT2[rows, g, tbs(qb)], qa[rows, c, tbs(qb)], True, True,
                       [B_KT2[g][t], B_qa[c][t]], [B_ps[si]])
                    p_, bp_ = pt_rot.next()
                    act(p_[:, lo:256], ps[si][:, lo:256], AF.Exp, [B_ps[si]], [bp_], scale=SCALE)
                    vtt(p_[:, lo:256], p_[:, lo:256], swam_bf[:, lo:256], ALU.mult, [bp_, B_tab], [bp_])
                    st["p"] = (p_, bp_)

                def s2():
                    p_, bp_ = st["p"]
                    if qb > 0:
                        mm(ps[po][rows, qcols], V[:, qb - 1, g * 64:(g + 1) * 64], p_[:, 0:128], True, False,
                           [B_V[qb - 1], bp_], [B_ps[po]])
                        mm(ps[pd][rows, qcols], ones_bf[:, 0:64], p_[:, 0:128], True, False, [B_tab, bp_], [B_ps[pd]])
                    mm(ps[po][rows, qcols], V[:, qb, g * 64:(g + 1) * 64], p_[:, 128:256], qb == 0, True,
                       [B_V[qb], bp_], [B_ps[po]])
                    mm(ps[pd][rows, qcols], ones_bf[:, 0:64], p_[:, 128:256], qb == 0, True, [B_tab, bp_], [B_ps[pd]])
                    if qb % 4 == 3:
                        normalize(po, pd, rows, qa[rows, c, tgs(t)], B_qa[c][t],
                                  sink_ap=sinkE[rows, j * 12 + h:j * 12 + h + 1])
                return (s1, s2)

            items = []
            for h in range(12):
                for t in range(NTG):
                    po, pd = acc_rot.next()
                    for qb in range(4 * t, 4 * t + 4):
                        items.append(swa_item(h, t, qb, po, pd))
            run_items(items, 3)
            mem_attention()
        else:
            wsrc = wfox_d[j]
            KTP = [aview(KV_OFF, 2048)]
            B_KTP = [[Buf(f"ktp{i}_{t}", B_phase) for t in range(NTG)] for i in range(1)]
            VP = [aview(KV_OFF + 2048, 4096).rearrange("p (b n) -> p b n", b=NTB)]
            B_VP = [[Buf(f"vp{i}_{t}", B_phase) for t in range(NTG)] for i in range(1)]
            pr.add("dve", lambda e, vp0=VP[0]: e.memset(vp0[:, :, 64:192], 1.0), [], B_VP[0])
            Dall = aview(KV_OFF + 6144, 2048)
            B_Dall = [Buf(f"Dall{t}", B_phase) for t in range(NTG)]
            pr.add("dve", lambda e, Dall=Dall: e.memset(Dall[:, :], 0.0), [], B_Dall)
            DTs = [ln_mean, ln_rstd]
            B_DTs = [B_lnm, B_lnr]
            for t in range(NTG):
                si = s_rot.next()
                for k in range(NCH):
                    mm(ps[si][0:12, :], spm[:, SP_WF + j * 96 + k * 12:SP_WF + j * 96 + (k + 1) * 12],
                       xres[:, k, tgs(t)], k == 0, k == NCH - 1, [B_const, B_xres[k][t]], [B_ps[si]])
                e1, be1 = tmp_rot.next()
                act(e1[0:12, :], ps[si][0:12, :], AF.Exp, [B_ps[si], B_tab], [be1],
                    bias=negbf[0:12, j:j + 1], scale=-1.0 / ALPHA)
                act(e1[0:12, :], e1[0:12, :], AF.Ln, [be1], [be1], bias=1.0)
                dt_, bdt_ = DTs[t % 2], B_DTs[t % 2]
                dtp, bdtp = DTs[(t + 1) % 2], B_DTs[(t + 1) % 2]
                if t == 0:
                    pr.add("dve", lambda e, e1=e1, dt_=dt_: e.tensor_tensor_scan(
                        dt_[0:12, :], ones_bf[0:12, :], e1[0:12, :], 0.0, ALU.mult, ALU.subtract),
                        [be1, B_tab], [bdt_])
                else:
                    pr.add("dve", lambda e, e1=e1, dt_=dt_, dtp=dtp: e.tensor_tensor_scan(
                        dt_[0:12, :], ones_bf[0:12, :], e1[0:12, :], dtp[0:12, 511:512],
                        ALU.mult, ALU.subtract), [be1, B_tab, bdtp], [bdt_])
                d8, bd8 = tmp_rot.next()
                vts(d8[0:12, :], dt_[0:12, :], 8.0, None, ALU.mult, None, [bdt_], [bd8])
                vcopy(Dall[0:12, tgs(t)], d8[0:12, :], [bd8], [B_Dall[t]])
                vcopy(Dall[64:76, tgs(t)], d8[0:12, :], [bd8], [B_Dall[t]])
                si2 = s_rot.next()
                for jb in range(4):
                    tr(ps[si2][:, jb * 12:(jb + 1) * 12], dt_[0:12, tbs(jb)], cp[0:12, CP_ID:CP_ID + 12],
                       [bdt_, B_const], [B_ps[si2]])
                act(negD[:, t * 4:(t + 1) * 4, :], ps[si2][:, 0:48].rearrange("p (b h) -> p b h", b=4), AF.Identity,
                    [B_ps[si2]], [B_negD[t]], scale=-1.0)
            wv, bw = wload_in(w_attn_rot, wsrc, [(2304, 256)])
            for c in (6, 7):
                for t in range(NTG):
                    si = s_rot.next()
                    proj_fm(wv, bw, (c - 6) * 128, t, si)
                    act(qa[:, c, tgs(t)], ps[si][:, :], AF.Identity, [B_ps[si]], [B_qa[c][t]])
            for c in range(6):
                slot = 0
                wv, bw = wload_in(w_attn_rot, wsrc, [(c * 128, 128), (768 + c * 128, 128), (1536 + c * 128, 128)])
                ktp = KTP[slot]
                vp = VP[slot]
                for t in range(NTG):
                    si = s_rot.next()
                    proj_fm(wv, bw, 0, t, si)
                    act(qa[:, c, tgs(t)], ps[si][:, :], AF.Identity, [B_ps[si]], [B_qa[c][t]])
                    si = s_rot.next()
                    proj_fm(wv, bw, 128, t, si)
                    vcopy(ktp[:, tgs(t)], ps[si][:, :], [B_ps[si]], [B_KTP[slot][t]])
                    si = s_rot.next()
                    for jb in range(4):
                        b = t * 4 + jb
                        for k in range(NCH):
                            mm(ps[si][:, jb * 128:(jb + 1) * 128], xb[:, k, tbs(b)], wv[:, k, 256:384], k == 0,
                               k == NCH - 1, [bw, B_xb[k][t]], [B_ps[si]])
                    psv = ps[si][:, :].rearrange("p (b n) -> p b n", b=4)
                    act(vp[:, t * 4:(t + 1) * 4, 0:64], psv[:, :, 0:64], AF.Identity, [B_ps[si]], [B_VP[slot][t]])
                    act(vp[:, t * 4:(t + 1) * 4, 192:256], psv[:, :, 64:128], AF.Identity, [B_ps[si]], [B_VP[slot][t]])
                def fox_item(c, t, kb, pos_, ktp, vp):
                    nkb = 4 * t + 4
                    jd = kb - 4 * t
                    c0 = max(jd, 0) * 128
                    last = (kb == nkb - 1)
                    st = {}
                    rws = [slice(0, 64), slice(64, 128)]

                    def s1():
                        sis = [s_rot.next(), s_rot.next()]
                        for r in range(2):
                            mm(ps[sis[r]][:, c0:512], ktp[rws[r], tbs(kb)], qa[rws[r], c, t * 512 + c0:(t + 1) * 512],
                               True, False, [B_KTP[0][kb // 4], B_qa[c][t]], [B_ps[sis[r]]])
                        for r in range(2):
                            h = 2 * c + r
                            mm(ps[sis[r]][:, c0:512], sel_bf[rws[r], h * 128:(h + 1) * 128],
                               Dall[rws[r], t * 512 + c0:(t + 1) * 512], False, True, [B_tab, B_Dall[t]], [B_ps[sis[r]]])
                        pp_ = []
                        for r in range(2):
                            h = 2 * c + r
                            p_, bp_ = pt_rot.next()
                            act(p_[:, c0:512], ps[sis[r]][:, c0:512], AF.Exp, [B_ps[sis[r]], B_negD[kb // 4]], [bp_],
                                bias=negD[:, kb, h:h + 1], scale=SCALE)
                            if jd >= 0:
                                vtt(p_[:, c0:c0 + 128], p_[:, c0:c0 + 128], tri_bf[:, :], ALU.mult, [bp_, B_tab], [bp_])
                            pp_.append((p_, bp_))
                        st["p"] = pp_

                    def s2():
                        for r in range(2):
                            p_, bp_ = st["p"][r]
                            po = pos_[r]
                            mm(ps[po][:, c0:512], vp[:, kb, r * 128:(r + 1) * 128], p_[:, c0:512], kb == 0, last,
                               [B_VP[0][kb // 4], bp_], [B_ps[po]])
                        if last:
                            for r in range(2):
                                po = pos_[r]
                                rows, drows = rws[r], rws[1 - r]
                                tmpn, btmpn = tmp_rot.next()
                                act(tmpn[drows, :], ps[po][drows, :], AF.Ln, [B_ps[po]], [btmpn])
                                act(tmpn[drows, :], tmpn[drows, :], AF.Exp, [btmpn], [btmpn], scale=-1.0)
                                vcopy(tmpn[rows, :], tmpn[drows, :], [btmpn], [btmpn])
                                vtt(qa[rows, c, tgs(t)], ps[po][rows, :], tmpn[rows, :], ALU.mult, [B_ps[po], btmpn],
                                    [B_qa[c][t]])
                    return (s1, s2)

                items = []
                for t in range(NTG):
                    pos_ = (fox_rot.next(), fox_rot.next())
                    for kb in range(4 * t + 4):
                        items.append(fox_item(c, t, kb, pos_, ktp, vp))
                run_items(items, 1)
            mem_attention()

        if ("attn%d" % l) in tap_d:
            td = tap_d["attn%d" % l]
            G_tap = pr.grp()
            for c in range(NCH):
                for t in range(NTG):
                    tt_, btt = tmp_rot.next()
                    vcopy(tt_[:, :], qa[:, c, tgs(t)], [B_qa[c][t]], [btt])
                    dma_sp(td[c * 128:(c + 1) * 128, tgs(t)], tt_[:, :], G_tap, [btt], [B_out])

        _chk("C%d" % l)
        wo_t = [wload_in(w_attn_rot, wout_d[l], [(hh * 512, 512)]) for hh in range(2)]
        gcol, bcol = (l * 4 + 0) * 8, (l * 4 + 1) * 8
        for t in range(NTG):
            for dc in range(NCH):
                wv, bw = wo_t[dc // 4]
                si = s_rot.next()
                for k in range(NCH):
                    mm(ps[si][:, :], wv[:, k, (dc % 4) * 128:(dc % 4 + 1) * 128], qa[:, k, tgs(t)], k == 0, k == NCH - 1,
                       [bw, B_qa[k][t]], [B_ps[si]])
                vtt(xres[:, dc, tgs(t)], xres[:, dc, tgs(t)], ps[si][:, :], ALU.add, [B_xres[dc][t], B_ps[si]],
                    [B_xres[dc][t]])
            layer_norm(t, gcol, bcol, gcol, bcol)

        if ("x1_%d" % l) in tap_d:
            td = tap_d["x1_%d" % l]
            G_tap = pr.grp()
            for c in range(NCH):
                for t in range(NTG):
                    dma_sp(td[c * 128:(c + 1) * 128, tgs(t)], xres[:, c, tgs(t)], G_tap, [B_xres[c][t]], [B_out])

        _chk("D%d" % l)
        phase_switch()

        def ffn_pass(w_in2d, w_out2d, gate):
            for f0 in range(0, NFC, 4):
                fcn = min(4, NFC - f0)
                wa, bwa = wload_in(w_ffn_rot, w_in2d, [(f0 * 128, fcn * 128)])
                wb, bwb = wload_in(w_ffn_rot, w_in2d, [(DFF + f0 * 128, fcn * 128)])
                wo, bwo = wload_rows(w_ffn_rot, w_out2d, f0, fcn, D)
                for t in range(NTG):
                    for fc in range(fcn):
                        sa_i, sb_i = s_rot.next(), s_rot.next()
                        proj_fm(wa, bwa, fc * 128, t, sa_i)
                        proj_fm(wb, bwb, fc * 128, t, sb_i)
                        sa, bsa = tmp_rot.next()
                        act(sa[:, :], ps[sa_i][:, :], AF.Silu, [B_ps[sa_i]], [bsa])
                        if gate is not None:
                            gt_, bgt_ = gate
                            vtt(sa[:, :], sa[:, :], gt_[:, tgs(t)], ALU.mult, [bsa, bgt_[t]], [bsa])
                        vtt(gp[:, fc, tgs(t)], sa[:, :], ps[sb_i][:, :], ALU.mult, [bsa, B_ps[sb_i]], [B_gp[fc][t]])
                for t in range(NTG):
                    for dc in range(NCH):
                        yi = 4 + (dc % 4)
                        for fc in range(fcn):
                            mm(ps[yi][:, :], wo[:, fc, dc * 128:(dc + 1) * 128], gp[:, fc, tgs(t)], fc == 0, fc == fcn - 1,
                               [bwo, B_gp[fc][t]], [B_ps[yi]])
                        vtt(xres[:, dc, tgs(t)], xres[:, dc, tgs(t)], ps[yi][:, :], ALU.add,
                            [B_xres[dc][t], B_ps[yi]], [B_xres[dc][t]])

        if swa:
            ffn_pass(wfi_d[j], wfo_d[j], None)
        else:
            for b in range(NTB):
                si = s_rot.next()
                for k in range(NCH):
                    mm(ps[si][:, 0:8], xres[:, k, tbs(b)], spm[:, SP_WR + j * 64 + k * 8:SP_WR + j * 64 + (k + 1) * 8],
                       k == 0, k == NCH - 1, [B_xres[k][b // 4], B_const], [B_ps[si]])
                sm, bsm = small_rot.next()
                lg = sm[:, 0:8]
                pr.add("dve", lambda e, lg=lg, si=si: e.scalar_tensor_tensor(
                    lg, ps[si][:, 0:8], 1.0 / ALPHA, spm[:, SP_BR + j * 8:SP_BR + (j + 1) * 8], ALU.mult, ALU.add),
                    [B_ps[si], B_const], [bsm])
                sm2, bsm2 = small_rot.next()
                mx = sm2[:, 0:8]
                pr.add("dve", lambda e, mx=mx, lg=lg: e.max(mx, lg), [bsm], [bsm2])
                dd = sm2[:, 8:9]
                vtt(dd, sm2[:, 1:2], sm2[:, 0:1], ALU.subtract, [bsm2], [bsm2])
                ee = sm2[:, 9:10]
                act(ee, dd, AF.Exp, [bsm2], [bsm2])
                ss = sm2[:, 10:11]
                vts(ss, ee, 1.0, None, ALU.add, None, [bsm2], [bsm2])
                g1 = sm2[:, 11:12]
                vrecip(g1, ss, [bsm2], [bsm2])
                g2 = sm2[:, 12:13]
                vtt(g2, ee, g1, ALU.mult, [bsm2], [bsm2])
                m1 = sm[:, 8:16]
                vts(m1, lg, sm2[:, 0:1], g1, ALU.is_equal, ALU.mult, [bsm, bsm2], [bsm])
                sm3, bsm3 = small_rot.next()
                m2 = sm3[:, 0:8]
                vts(m2, lg, sm2[:, 1:2], g2, ALU.is_equal, ALU.mult, [bsm, bsm2], [bsm3])
                vtt(Gt[:, b, :], m1, m2, ALU.add, [bsm, bsm3], [B_Gt[b]])
            if ("gate%d" % l) in tap_d:
                td = tap_d["gate%d" % l]
                G_tap = pr.grp()
                for b in range(NTB):
                    dma_sp(td[tbs(b), :], Gt[:, b, :], G_tap, [B_Gt[b]], [B_out])
            for ex in range(8):
                for t in range(NTG):
                    si = s_rot.next()
                    for jb in range(4):
                        b = t * 4 + jb
                        dg, bdg = tmp_rot.next()
                        vts(dg[:, 0:128], ident, Gt[:, b, ex:ex + 1], None, ALU.mult, None, [B_const, B_Gt[b]], [bdg])
                        mm(ps[si][:, jb * 128:(jb + 1) * 128], cp[:, CP_ONES:CP_ONES + 128],
                           dg[:, 0:128], True, True, [B_const, bdg], [B_ps[si]])
                    act(GB[:, tgs(t)], ps[si][:, :], AF.Identity, [B_ps[si]], [B_GB[t]])
                ffn_pass(wei_d[j, ex], weo_d[j, ex], (GB, B_GB))

        gcol, bcol = (l * 4 + 2) * 8, (l * 4 + 3) * 8
        for t in range(NTG):
            layer_norm(t, gcol, bcol, gcol, bcol)
        if ("x2_%d" % l) in tap_d:
            td = tap_d["x2_%d" % l]
            G_tap = pr.grp()
            for c in range(NCH):
                for t in range(NTG):
                    dma_sp(td[c * 128:(c + 1) * 128, tgs(t)], xres[:, c, tgs(t)], G_tap, [B_xres[c][t]], [B_out])

    try:
        _layers()
    except _Stop:
        pass

    phase_switch()
    ps_rot2 = Rot([(ps[i], B_ps[i]) for i in range(8)])
    k_alt = 0
    for b in range(NTB):
        sj = b % 4
        for half in range(2):
            pp, bpp = ps_rot2.next()
            for cc in range(4):
                c = half * 4 + cc
                tr(pp[:, cc * 128:(cc + 1) * 128], xres[:, c, tbs(b)], ident, [B_xres[c][b // 4], B_const], [bpp])
            if k_alt % 2 == 0:
                act(stg[sj][:, half * 512:(half + 1) * 512], pp[:, :], AF.Identity, [bpp], [B_stg[sj]])
            else:
                vcopy(stg[sj][:, half * 512:(half + 1) * 512], pp[:, :], [bpp], [B_stg[sj]])
            k_alt += 1
        dma_sp(out_d[tbs(b), :], stg[sj], G_stg[sj], [B_stg[sj]], [B_outs[b]])
    fin_reads = [B_out] + B_outs + B_stg
    pr.add("sp", lambda e: None, fin_reads, fin_reads)

    pr.emit(nc, es)
    es.close()
    return nc


_CACHE = {}


def _prep_inputs(inp):
    cols = _swa_cols()
    w_swa = np.ascontiguousarray(inp["w_in_swa"][:, :, cols])
    shared = {
        "w_swa": w_swa,
        "w_fox": np.ascontiguousarray(inp["w_in_fox"]),
        "w_kv": np.ascontiguousarray(inp["w_mem_kv"]),
        "w_out": np.ascontiguousarray(inp["w_out"]),
        "w_ffn_in": np.ascontiguousarray(inp["w_ffn_in"]),
        "w_ffn_out": np.ascontiguousarray(inp["w_ffn_out"]),
        "w_exp_in": np.ascontiguousarray(inp["w_exp_in"]),
        "w_exp_out": np.ascontiguousarray(inp["w_exp_out"]),
        "sp": _make_sp(inp),
        "cp": _make_consts()[0],
        "cx": _make_consts()[1],
    }
    return shared


def kernel(**inputs):
    inp = {k: np.asarray(v) for k, v in inputs.items()}
    n = 8
    if "nc" not in _CACHE:
        _CACHE["nc"] = build(4)
    nc = _CACHE["nc"]
    shared = _prep_inputs(inp)
    in_maps = []
    for b in range(n):
        m = dict(shared)
        m["x"] = np.ascontiguousarray(inp["x"][b])
        m["mem"] = np.ascontiguousarray(inp["mem"][b])
        m["pos"] = np.ascontiguousarray(inp["positions"][b].reshape(1, S).astype(np.int32))
        in_maps.append(m)
    res = run_bass_kernel_spmd(nc, in_maps, core_ids=list(range(n)))
    return np.stack([np.asarray(r["out"]) for r in res.results], axis=0).astype(np.float32)
```

```python
import numpy as np
from contextlib import ExitStack
import concourse.bass as bass
import concourse.mybir as mybir
from concourse.bass_utils import run_bass_kernel_spmd

F32 = mybir.dt.float32
BF16 = mybir.dt.bfloat16
I32 = mybir.dt.int32
AF = mybir.ActivationFunctionType
ALU = mybir.AluOpType

S = 2048
D = 1024
NCH = 8
NTG = 4
NTB = 16
DFF = 2816
NFC = 22
ALPHA = 8.0 ** 0.25
SCALE = 0.125
EPOCH = 12000
PI = float(np.pi)


class Buf:
    __slots__ = ("name", "lw", "rd", "phase")

    def __init__(self, name, phase=None):
        self.name = name
        self.lw = None
        self.rd = []
        self.phase = phase


class SemGrp:
    __slots__ = ("sem", "cnt", "batch", "idx")

    def __init__(self, batch=False):
        self.sem = None
        self.cnt = 0
        self.batch = batch


class Op:
    __slots__ = ("eng", "fn", "deps", "sig", "grp", "count", "dma", "epoch")


class Prog:
    def __init__(self):
        self.ops = []
        self.grps = []

    def grp(self, batch=False):
        g = SemGrp(batch)
        self.grps.append(g)
        return g

    def add(self, eng, fn, reads=(), writes=(), dma=None):
        op = Op()
        op.eng = eng
        op.fn = fn
        op.dma = dma is not None
        op.grp = dma
        op.sig = op.dma
        op.count = 0
        op.epoch = 0
        reads = list(reads)
        writes = list(writes)
        for b in list(reads) + list(writes):
            if b.phase is not None:
                reads.append(b.phase)
        deps = {}
        for b in reads:
            if b.lw is not None:
                deps[id(b.lw)] = (b.lw, True)
        for b in writes:
            if b.lw is not None and id(b.lw) not in deps:
                deps[id(b.lw)] = (b.lw, False)
            for r in b.rd:
                if id(r) not in deps:
                    deps[id(r)] = (r, False)
        fin = []
        for p, raw in deps.values():
            if p is op:
                continue
            if op.dma and p.dma and p.grp is op.grp and op.grp.batch:
                continue
            if (not op.dma) and (not p.dma) and p.eng == eng:
                if eng == "pe":
                    continue
            fin.append(p)
            p.sig = True
        op.deps = fin
        for b in reads:
            if not op.dma:
                b.rd = [r for r in b.rd if r.dma or r.eng != eng]
            b.rd.append(op)
        for b in writes:
            b.lw = op
            b.rd = []
        self.ops.append(op)
        return op

    def emit(self, nc, es):
        engs = ["pe", "act", "dve", "pool", "sp"]
        ecount = {e: 0 for e in engs}
        eepoch = {e: 0 for e in engs}
        for op in self.ops:
            if op.dma:
                op.grp.cnt += 16
                op.count = op.grp.cnt
            elif op.sig:
                if ecount[op.eng] >= EPOCH:
                    ecount[op.eng] = 0
                    eepoch[op.eng] += 1
                ecount[op.eng] += 1
                op.count = ecount[op.eng]
                op.epoch = eepoch[op.eng]
        for gi, g in enumerate(self.grps):
            if g.cnt > 0:
                g.sem = es.enter_context(nc.semaphore(f"sg{gi}"))
        esem = {}
        for e in engs:
            for k in range(eepoch[e] + 1):
                esem[(e, k)] = es.enter_context(nc.semaphore(f"se_{e}_{k}"))
        for op in self.ops:
            if op.dma and op.grp.batch:
                op.count = op.grp.cnt
        block = es.enter_context(nc.Block())
        streams = {e: [o for o in self.ops if o.eng == e] for e in engs}

        def run(engname, eobj):
            waited = {}
            for op in streams[engname]:
                need = {}
                for p in op.deps:
                    if p.dma:
                        key = ("g", id(p.grp))
                        sem = p.grp.sem
                    else:
                        key = (p.eng, p.epoch)
                        sem = esem[key]
                    if waited.get(key, 0) >= p.count:
                        continue
                    if key not in need or need[key][1] < p.count:
                        need[key] = (sem, p.count)
                for key, (sem, cnt) in need.items():
                    eobj.wait_ge(sem, cnt)
                    waited[key] = cnt
                ins = op.fn(eobj)
                if ins is None:
                    continue
                if op.dma:
                    ins.then_inc(op.grp.sem, 16)
                elif op.sig:
                    ins.then_inc(esem[(op.eng, op.epoch)], 1)

        if streams["pe"]:
            @block.tensor
            def _(e):
                run("pe", e)

        if streams["act"]:
            @block.scalar
            def _(e):
                run("act", e)

        if streams["dve"]:
            @block.vector
            def _(e):
                run("dve", e)

        if streams["pool"]:
            @block.gpsimd
            def _(e):
                run("pool", e)

        if streams["sp"]:
            @block.sync
            def _(e):
                run("sp", e)


class Rot:
    def __init__(self, items):
        self.items = items
        self.i = 0

    def next(self):
        it = self.items[self.i % len(self.items)]
        self.i += 1
        return it


NCP = 128 + 128 + 128 + 3
CP_ID, CP_ONESM, CP_ONES, CP_INVF = 0, 128, 256, 384
CP_SIGN, CP_EPS = CP_INVF + 1, CP_INVF + 2
NCX = 128 + 256 + 12 * 128 + 512
CX_TRI, CX_SWAM, CX_SEL, CX_ONES = 0, 128, 384, 384 + 1536
NSP = 128 + 24 + 16 + 2 + 128 + 192
SP_LN, SP_SINK, SP_BR, SP_BF, SP_WR, SP_WF = 0, 128, 152, 168, 170, 298
SWA_EXT = 2752


def _swap16(d):
    return d + 8 if d < 8 else (d - 8 if d < 16 else d)


def _make_consts():
    cp = np.zeros((128, NCP), np.float32)
    cx = np.zeros((128, NCX), np.float32)
    cp[:, CP_ID:CP_ID + 128] = np.eye(128, dtype=np.float32)
    cp[:, CP_ONESM:CP_ONESM + 128] = 1.0 / 1024.0
    k = np.arange(128)[:, None]
    q = np.arange(128)[None, :]
    cx[:, CX_TRI:CX_TRI + 128] = (q >= k).astype(np.float32)
    mp = np.ones((128, 128), np.float32)
    mp[:64, 64:] = 0.0
    mo = np.ones((128, 128), np.float32)
    mo[64:, :64] = 0.0
    cx[:, CX_SWAM:CX_SWAM + 128] = mp
    cx[:, CX_SWAM + 128:CX_SWAM + 256] = mo
    cp[:, CP_ONES:CP_ONES + 128] = 1.0
    cx[:, CX_ONES:CX_ONES + 512] = 1.0
    for h in range(12):
        cx[h, CX_SEL + h * 128:CX_SEL + (h + 1) * 128] = 1.0
        cx[64 + h, CX_SEL + h * 128:CX_SEL + (h + 1) * 128] = 1.0
    inv_freq = (500000.0 ** (-np.arange(0, 16, 2, dtype=np.float32) / 16)).astype(np.float32)
    for p in range(128):
        d = p % 64
        if d < 16:
            cp[p, CP_INVF] = inv_freq[d % 8]
            cp[p, CP_SIGN] = -1.0 if d < 8 else 1.0
    cp[:, CP_EPS] = 1e-5
    return cp, cx


def _make_sp(inp):
    sp = np.zeros((128, NSP), np.float32)
    vecs = [inp["ln_attn_g"], inp["ln_attn_b"], inp["ln_ffn_g"], inp["ln_ffn_b"]]
    for l in range(4):
        for j in range(4):
            sp[:, SP_LN + (l * 4 + j) * 8:SP_LN + (l * 4 + j) * 8 + 8] = vecs[j][l].reshape(8, 128).T
    sp[:, SP_SINK:SP_SINK + 24] = inp["attn_sinks"].reshape(1, 24)
    sp[:, SP_BR:SP_BR + 16] = inp["b_router"].reshape(1, 16)
    sp[:12, SP_BF:SP_BF + 2] = inp["b_forget"].T
    for j in range(2):
        sp[:, SP_WR + j * 64:SP_WR + (j + 1) * 64] = (
            inp["w_router"][j].reshape(8, 128, 8).transpose(1, 0, 2).reshape(128, 64))
        sp[:, SP_WF + j * 96:SP_WF + (j + 1) * 96] = (
            inp["w_in_fox"][j][:, 2560:2572].reshape(8, 128, 12).transpose(1, 0, 2).reshape(128, 96))
    return sp


def _swa_cols():
    main = list(range(768))
    for g in range(3):
        for r in range(2):
            main += [768 + g * 64 + d for d in range(64)]
    main += list(range(960, 1408))
    sw = []
    for h in range(12):
        sw += [h * 64 + _swap16(d) for d in range(64)]
    for g in range(3):
        for r in range(2):
            sw += [768 + g * 64 + _swap16(d) for d in range(64)]
    return np.array(main + sw, np.int64)


def build(n_layers=4, taps=(), stop_after=None):
    nc = bass.Bass("TRN2", target_bir_lowering=False)
    pr = Prog()
    es = ExitStack()

    def din(name, shape, dt=F32):
        return nc.dram_tensor(name, list(shape), dt, kind="ExternalInput").ap()

    x_d = din("x", [S, D])
    mem_d = din("mem", [256, D])
    pos_d = din("pos", [1, S], I32)
    if n_layers >= 1:
        wswa_d = din("w_swa", [2, D, SWA_EXT])
        wkv_d = din("w_kv", [4, D, 512])
        wout_d = din("w_out", [4, D, D])
        wfi_d = din("w_ffn_in", [2, D, 2 * DFF])
        wfo_d = din("w_ffn_out", [2, DFF, D])
    if n_layers >= 2:
        wfox_d = din("w_fox", [2, D, 2572])
        wei_d = din("w_exp_in", [2, 8, D, 2 * DFF])
        weo_d = din("w_exp_out", [2, 8, DFF, D])
    sp_d = din("sp", [128, NSP])
    cp_d = din("cp", [128, NCP])
    cx_d = din("cx", [128, NCX])
    out_d = nc.dram_tensor("out", [S, D], F32, kind="ExternalOutput").ap()
    tap_d = {}
    for name, shape in taps:
        tap_d[name] = nc.dram_tensor(name, list(shape), F32, kind="ExternalOutput").ap()

    def sb(name, shape, dt):
        return es.enter_context(nc.sbuf_tensor("s_" + name, list(shape), dt))

    xres = sb("xres", [128, NCH, S], F32)
    xb = sb("xb", [128, NCH, S], BF16)
    arena = sb("arena", [128, 25600], BF16)
    wbase = [sb(f"wbase{i}", [128, 4096], BF16) for i in range(2)]
    Ctab = sb("Ctab", [128, S], BF16)
    Stab = sb("Stab", [128, S], BF16)
    cp = sb("cp", [128, NCP], F32)
    spm = sb("spm", [128, NSP], F32)
    lnps = sb("lnps", [128, 128], F32)
    sinkE = sb("sinkE", [128, 24], F32)
    negbf = sb("negbf", [128, 2], F32)
    tri_bf = sb("tri_bf", [128, 128], BF16)
    swam_bf = sb("swam_bf", [128, 256], BF16)
    ones_bf = sb("ones_bf", [128, 512], BF16)
    sel_bf = sb("sel_bf", [128, 1536], BF16)
    memT = sb("memT", [128, NCH, 256], BF16)
    kmT = sb("kmT", [128, 2, 256], BF16)
    vm = sb("vm", [128, 2, 256], BF16)
    negD = sb("negD", [128, NTB, 12], F32)
    Gt = sb("Gt", [128, NTB, 8], F32)
    tmpf = [sb(f"tmpf{i}", [128, 512], F32) for i in range(6)]
    onesm_bf = sb("onesm_bf", [128, 128], BF16)
    ln_mean = sb("ln_mean", [128, 512], F32)
    ln_rstd = sb("ln_rstd", [128, 512], F32)
    B_lnm = Buf("ln_mean")
    B_lnr = Buf("ln_rstd")
    ptb = [sb(f"pt{i}", [128, 512], BF16) for i in range(4)]
    smallf = [sb(f"small{i}", [128, 16], F32) for i in range(8)]
    ps = [es.enter_context(nc.psum_tensor(f"p_ps{i}", [128, 512], F32)) for i in range(8)]

    B_xres = [[Buf(f"xres{c}_{t}") for t in range(NTG)] for c in range(NCH)]
    B_xb = [[Buf(f"xb{c}_{t}") for t in range(NTG)] for c in range(NCH)]
    B_phase = Buf("phase")
    B_ps = [Buf(f"ps{i}") for i in range(8)]
    B_tmpf = [Buf(f"tmpf{i}") for i in range(6)]
    B_pt = [Buf(f"pt{i}") for i in range(4)]
    B_small = [Buf(f"small{i}") for i in range(8)]
    B_wbase = [Buf(f"wbase{i}") for i in range(2)]
    G_wbase = [pr.grp() for _ in range(2)]
    B_const = Buf("const")
    B_tab = Buf("tab")
    B_memT = Buf("memT")
    B_kmT = Buf("kmT")
    B_vm = Buf("vm")
    B_negD = [Buf(f"negD{t}") for t in range(NTG)]
    B_Gt = [Buf(f"Gt{t}") for t in range(NTB)]
    B_out = Buf("outdram")
    B_outs = [Buf(f"outdram{b}") for b in range(NTB)]
    G_const = pr.grp(batch=True)

    tmp_rot = Rot(list(zip(tmpf, B_tmpf)))
    pt_rot = Rot(list(zip(ptb, B_pt)))
    small_rot = Rot(list(zip(smallf, B_small)))

    def tgs(t):
        return slice(t * 512, (t + 1) * 512)

    def tbs(t):
        return slice(t * 128, (t + 1) * 128)

    def mm(out, lhsT, rhs, start, stop, reads, writes):
        pr.add("pe", lambda e: e.matmul(out, lhsT, rhs, start=start, stop=stop), reads, writes)

    def tr(out, in_, ident, reads, writes):
        pr.add("pe", lambda e: e.transpose(out, in_, ident), reads, writes)

    def act(out, in_, func, reads, writes, bias=None, scale=None):
        kw = {}
        if bias is not None:
            kw["bias"] = bias
        if scale is not None:
            kw["scale"] = scale
        pr.add("act", lambda e: e.activation(out, in_, func, **kw), reads, writes)

    def vtt(out, a, b, op, reads, writes):
        pr.add("dve", lambda e: e.tensor_tensor(out, a, b, op), reads, writes)

    def vts(out, a, s1, s2, op0, op1, reads, writes):
        if op1 is None:
            pr.add("dve", lambda e: e.tensor_scalar(out, a, s1, None, op0), reads, writes)
        else:
            pr.add("dve", lambda e: e.tensor_scalar(out, a, s1, s2, op0, op1), reads, writes)

    def vcopy(out, a, reads, writes):
        pr.add("dve", lambda e: e.tensor_copy(out, a), reads, writes)

    def vrecip(out, a, reads, writes):
        pr.add("dve", lambda e: e.reciprocal(out, a), reads, writes)

    def dma_sp(out, in_, grp, reads, writes):
        pr.add("sp", lambda e: e.dma_start(out=out, in_=in_), reads, writes, dma=grp)

    def dma_pool(out, in_, grp, reads, writes):
        pr.add("pool", lambda e: e.dma_start(out=out, in_=in_), reads, writes, dma=grp)

    def phase_switch():
        sm, bsm = small_rot.next()
        pr.add("dve", lambda e: e.memset(sm[:, 0:1], 0.0), [], [bsm, B_phase])

    def aview(off, n):
        return arena[:, off:off + n]

    QA_OFF = 0
    KV_OFF = 16384
    qa = aview(QA_OFF, 16384).rearrange("p (c t) -> p c t", c=NCH)
    B_qa = [[Buf(f"qa{c}_{t}", B_phase) for t in range(NTG)] for c in range(NCH)]
    gp = aview(0, 8192).rearrange("p (c t) -> p c t", c=4)
    B_gp = [[Buf(f"gp{c}_{t}", B_phase) for t in range(NTG)] for c in range(4)]
    GB = aview(8192, 4096).bitcast(F32)
    B_GB = [Buf(f"GB{t}", B_phase) for t in range(NTG)]
    wext = [aview(12288 + i * 4096, 4096) for i in range(3)]
    B_wext = [Buf(f"wext{i}", B_phase) for i in range(3)]
    G_wext = [pr.grp() for _ in range(3)]
    stg = [aview(i * 2048, 2048).bitcast(F32) for i in range(4)]
    B_stg = [Buf(f"stg{i}", B_phase) for i in range(4)]
    G_stg = [pr.grp() for _ in range(4)]

    w_attn_rot = Rot([(wbase[i][:, :], B_wbase[i], G_wbase[i]) for i in range(2)])
    w_ffn_rot = Rot([(wbase[i][:, :], B_wbase[i], G_wbase[i]) for i in range(2)]
                    + [(wext[i], B_wext[i], G_wext[i]) for i in range(3)])

    def wload_in(rot, src2d, col_list):
        wt, bw, gw = rot.next()
        ntot = sum(n for _, n in col_list)
        v = wt[:, 0:8 * ntot].rearrange("p (k n) -> p k n", k=8)
        o = 0
        for c0, n in col_list:
            dma_pool(v[:, :, o:o + n], src2d[:, c0:c0 + n].rearrange("(k p) n -> p k n", p=128), gw, [], [bw])
            o += n
        return v, bw

    def wload_rows(rot, src2d, r0, nchunks, ncols):
        wt, bw, gw = rot.next()
        v = wt[:, 0:nchunks * ncols].rearrange("p (k n) -> p k n", k=nchunks)
        dma_pool(v, src2d[r0 * 128:(r0 + nchunks) * 128, :].rearrange("(k p) n -> p k n", p=128), gw, [], [bw])
        return v, bw

    ident = cp[:, CP_ID:CP_ID + 128]
    onesm = cp[:, CP_ONESM:CP_ONESM + 128]
    eps_ap = cp[:, CP_EPS:CP_EPS + 1]

    dma_sp(cp[:, :], cp_d, G_const, [], [B_const])
    dma_sp(spm[:, :], sp_d, G_const, [], [B_const])
    cxs = aview(8192, 2 * NCX).bitcast(F32)
    B_cxs = Buf("cxs", B_phase)
    G_cxs = pr.grp()
    if cx_d is not None:
        dma_sp(cxs, cx_d, G_cxs, [], [B_cxs])
        vcopy(tri_bf[:, :], cxs[:, CX_TRI:CX_TRI + 128], [B_cxs], [B_tab])
        vcopy(swam_bf[:, :], cxs[:, CX_SWAM:CX_SWAM + 256], [B_cxs], [B_tab])
        vcopy(ones_bf[:, :], cxs[:, CX_ONES:CX_ONES + 512], [B_cxs], [B_tab])
        vcopy(sel_bf[:, :], cxs[:, CX_SEL:CX_SEL + 1536], [B_cxs], [B_tab])
    vts(lnps[:, :], spm[:, SP_LN:SP_LN + 128], ALPHA, None, ALU.mult, None, [B_const], [B_tab])
    lastl = n_layers - 1
    if lastl >= 0:
        c0 = (lastl * 4 + 2) * 8
        vcopy(lnps[:, c0:c0 + 16], spm[:, SP_LN + c0:SP_LN + c0 + 16], [B_const, B_tab], [B_tab])
    act(sinkE[:, :], spm[:, SP_SINK:SP_SINK + 24], AF.Exp, [B_const], [B_tab])
    vts(negbf[:, :], spm[:, SP_BF:SP_BF + 2], -1.0, None, ALU.mult, None, [B_const], [B_tab])
    vcopy(onesm_bf[:, :], cp[:, CP_ONESM:CP_ONESM + 128], [B_const], [B_tab])

    G_pos = pr.grp()
    for t in range(NTG):
        pt_, bpt = tmpf[0], B_tmpf[0]
        pi_ = pt_[:, :].bitcast(I32)
        dma_sp(pi_, pos_d[0:1, tgs(t)].partition_broadcast(128), G_pos, [], [bpt])
        pf, bpf = tmpf[1], B_tmpf[1]
        vcopy(pf[:, :], pi_, [bpt], [bpf])
        ang, bang = tmpf[2], B_tmpf[2]
        vts(ang[:, :], pf[:, :], cp[:, CP_INVF:CP_INVF + 1], None, ALU.mult, None, [bpf, B_const], [bang])
        for which in range(2):
            a2, ba2 = tmpf[3], B_tmpf[3]
            if which == 1:
                vts(a2[:, :], ang[:, :], PI / 2, None, ALU.add, None, [bang], [ba2])
            else:
                vcopy(a2[:, :], ang[:, :], [bang], [ba2])
            u, bu = tmpf[4], B_tmpf[4]
            vts(u[:, :], a2[:, :], 1.0 / (2 * PI), 0.5, ALU.mult, ALU.add, [ba2], [bu])
            ui = pt_[:, :].bitcast(I32)
            vcopy(ui, u[:, :], [bu], [bpt])
            vcopy(u[:, :], ui, [bpt], [bu])
            C1, C2, C3 = 6.28125, 0.0019350051879882812, 3.019916050561005e-07
            for cc in (C1, C2, C3):
                pr.add("dve", (lambda cc_: lambda e: e.scalar_tensor_tensor(a2[:, :], u[:, :], -cc_, a2[:, :], ALU.mult, ALU.add))(cc),
                       [bu, ba2], [ba2])
            m, bm = tmpf[5], B_tmpf[5]
            vts(m[:, :], a2[:, :], PI, -2 * PI, ALU.is_gt, ALU.mult, [ba2], [bm])
            vtt(a2[:, :], a2[:, :], m[:, :], ALU.add, [ba2, bm], [ba2])
            vts(m[:, :], a2[:, :], -PI, 2 * PI, ALU.is_lt, ALU.mult, [ba2], [bm])
            vtt(a2[:, :], a2[:, :], m[:, :], ALU.add, [ba2, bm], [ba2])
            vts(a2[:, :], a2[:, :], PI, -PI, ALU.min, ALU.max, [ba2], [ba2])
            if which == 1:
                act(Ctab[:, tgs(t)], a2[:, :], AF.Sin, [ba2], [B_tab])
            else:
                act(a2[:, :], a2[:, :], AF.Sin, [ba2], [ba2])
                vts(Stab[:, tgs(t)], a2[:, :], cp[:, CP_SIGN:CP_SIGN + 1], None, ALU.mult, None, [ba2, B_const], [B_tab])

    ps_rot = Rot([(ps[i], B_ps[i]) for i in range(4)])
    stg_t = [(tmpf[0], tmpf[1]), (tmpf[2], tmpf[3]), (tmpf[4], tmpf[5]), (ln_mean, ln_rstd)]
    stg_b = [(B_tmpf[0], B_tmpf[1]), (B_tmpf[2], B_tmpf[3]), (B_tmpf[4], B_tmpf[5]), (B_lnm, B_lnr)]
    G_st2 = [[pr.grp() for _ in range(2)] for _ in range(4)]
    for t in range(NTG):
        for j in range(4):
            for hf in range(2):
                dma_sp(stg_t[j][hf][:, :], x_d[tbs(t * 4 + j), hf * 512:(hf + 1) * 512], G_st2[j][hf], [], [stg_b[j][hf]])
        for c in range(NCH):
            pp, bpp = ps_rot.next()
            hf = c // 4
            for j in range(4):
                tr(pp[:, j * 128:(j + 1) * 128], stg_t[j][hf][:, (c % 4) * 128:(c % 4 + 1) * 128], ident,
                   [stg_b[j][hf], B_const], [bpp])
            act(xres[:, c, tgs(t)], pp[:, :], AF.Identity, [bpp], [B_xres[c][t]], scale=ALPHA)
            act(xb[:, c, tgs(t)], pp[:, :], AF.Identity, [bpp], [B_xb[c][t]])
    for j in range(2):
        for hf in range(2):
            dma_sp(stg_t[j][hf][:, :], mem_d[tbs(j), hf * 512:(hf + 1) * 512], G_st2[j][hf], [], [stg_b[j][hf]])
    for c in range(NCH):
        pp, bpp = ps_rot.next()
        hf = c // 4
        for j in range(2):
            tr(pp[:, j * 128:(j + 1) * 128], stg_t[j][hf][:, (c % 4) * 128:(c % 4 + 1) * 128], ident,
               [stg_b[j][hf], B_const], [bpp])
        vcopy(memT[:, c, :], pp[:, 0:256], [bpp], [B_memT])

    def layer_norm(t, gcol, bcol, gscol, bscol):
        pm, bpm = ps[4], B_ps[4]
        pe2, bpe2 = ps[5], B_ps[5]
        for c in range(NCH):
            mm(pm[:, :], onesm, xres[:, c, tgs(t)], c == 0, c == NCH - 1, [B_const, B_xres[c][t]], [bpm])
        for c in range(NCH):
            sq, bsq = pt_rot.next()
            act(sq[:, :], xres[:, c, tgs(t)], AF.Square, [B_xres[c][t]], [bsq])
            mm(pe2[:, :], onesm_bf[:, :], sq[:, :], c == 0, c == NCH - 1, [B_tab, bsq], [bpe2])
        mean, bmean = ln_mean, B_lnm
        act(mean[:, :], pm[:, :], AF.Identity, [bpm], [bmean])
        msq, bmsq = tmp_rot.next()
        act(msq[:, :], pm[:, :], AF.Square, [bpm], [bmsq])
        var, bvar = tmp_rot.next()
        vtt(var[:, :], pe2[:, :], msq[:, :], ALU.subtract, [bpe2, bmsq], [bvar])
        act(var[:, :], var[:, :], AF.Ln, [bvar, B_const], [bvar], bias=eps_ap)
        rstd, brstd = ln_rstd, B_lnr
        act(rstd[:, :], var[:, :], AF.Exp, [bvar], [brstd], scale=-0.5)
        for c in range(NCH):
            u, bu = tmp_rot.next()
            vtt(u[:, :], xres[:, c, tgs(t)], mean[:, :], ALU.subtract, [B_xres[c][t], bmean], [bu])
            vtt(u[:, :], u[:, :], rstd[:, :], ALU.mult, [bu, brstd], [bu])
            act(xres[:, c, tgs(t)], u[:, :], AF.Identity, [bu, B_tab], [B_xres[c][t]],
                bias=lnps[:, bscol + c:bscol + c + 1], scale=lnps[:, gscol + c:gscol + c + 1])
            act(xb[:, c, tgs(t)], u[:, :], AF.Identity, [bu, B_const], [B_xb[c][t]],
                bias=spm[:, SP_LN + bcol + c:SP_LN + bcol + c + 1], scale=spm[:, SP_LN + gcol + c:SP_LN + gcol + c + 1])

    acc_rot = Rot([(4, 5), (6, 7)])
    fox_rot = Rot([4, 5, 6, 7])
    s_rot = Rot([0, 1, 2, 3])

    def normalize(po, pd, rows, dst_ap, dst_buf, sink_ap=None):
        tmp, btmp = tmp_rot.next()
        if sink_ap is not None:
            act(tmp[rows, :], ps[pd][rows, :], AF.Ln, [B_ps[pd], B_tab], [btmp], bias=sink_ap)
        else:
            act(tmp[rows, :], ps[pd][rows, :], AF.Ln, [B_ps[pd]], [btmp])
        act(tmp[rows, :], tmp[rows, :], AF.Exp, [btmp], [btmp], scale=-1.0)
        vtt(dst_ap, ps[po][rows, :], tmp[rows, :], ALU.mult, [B_ps[po], btmp], [dst_buf])

    def run_items(items, depth=2):
        n = len(items)
        for i in range(min(depth, n)):
            items[i][0]()
        for i in range(n):
            if i + depth < n:
                items[i + depth][0]()
            items[i][1]()

    class _Stop(Exception):
        pass

    def _chk(tag):
        if stop_after == tag:
            raise _Stop()

    def _layers():
      for l in range(n_layers):
        _layer(l)

    def _layer(l):
        j = l // 2
        swa = (l % 2 == 0)
        phase_switch()
        wkv, bwkv = wload_in(w_attn_rot, wkv_d[l], [(0, 512)])
        for c in range(2):
            si = s_rot.next()
            for k in range(NCH):
                mm(ps[si][:, 0:256], wkv[:, k, c * 128:(c + 1) * 128], memT[:, k, :], k == 0, k == NCH - 1,
                   [bwkv, B_memT], [B_ps[si]])
            act(kmT[:, c, :], ps[si][:, 0:256], AF.Identity, [B_ps[si]], [B_kmT])
        for mb in range(2):
            si = s_rot.next()
            for k in range(NCH):
                mm(ps[si][:, 0:256], memT[:, k, tbs(mb)], wkv[:, k, 256:512], k == 0, k == NCH - 1,
                   [bwkv, B_memT], [B_ps[si]])
            act(vm[:, mb, :], ps[si][:, 0:256], AF.Identity, [B_ps[si]], [B_vm])

        _chk("A%d" % l)

        def proj_fm(wv, bw, col0, t, si):
            for k in range(NCH):
                mm(ps[si][:, :], wv[:, k, col0:col0 + 128], xb[:, k, tgs(t)], k == 0, k == NCH - 1,
                   [bw, B_xb[k][t]], [B_ps[si]])

        def mem_item(m, t, mb, po, pd):
            c = 6 + m // 2
            r = m % 2
            rows = slice(r * 64, (r + 1) * 64)
            st = {}

            def s1():
                si = s_rot.next()
                mm(ps[si][:, :], kmT[rows, m // 2, tbs(mb)], qa[rows, c, tgs(t)], True, True,
                   [B_kmT, B_qa[c][t]], [B_ps[si]])
                p_, bp_ = pt_rot.next()
                act(p_[:, :], ps[si][:, :], AF.Exp, [B_ps[si]], [bp_], scale=SCALE)
                st["p"] = (p_, bp_)

            def s2():
                p_, bp_ = st["p"]
                mm(ps[po][rows, :], vm[:, mb, m * 64:(m + 1) * 64], p_[:, :], mb == 0, mb == 1,
                   [B_vm, bp_], [B_ps[po]])
                mm(ps[pd][rows, :], ones_bf[:, 0:64], p_[:, :], mb == 0, mb == 1, [B_tab, bp_], [B_ps[pd]])
                if mb == 1:
                    normalize(po, pd, rows, qa[rows, c, tgs(t)], B_qa[c][t])
            return (s1, s2)

        def mem_attention():
            items = []
            for m in range(4):
                for t in range(NTG):
                    po, pd = acc_rot.next()
                    for mb in range(2):
                        items.append(mem_item(m, t, mb, po, pd))
            run_items(items, 2)

        if swa:
            KT2 = aview(KV_OFF, 6144).rearrange("p (g t) -> p g t", g=3)
            B_KT2 = [[Buf(f"kt2_{g}_{t}", B_phase) for t in range(NTG)] for g in range(3)]
            V = aview(KV_OFF + 6144, 3072).rearrange("p (b n) -> p b n", b=NTB)
            B_V = [Buf(f"v{b}", B_phase) for b in range(NTB)]
            wsrc = wswa_d[j]
            for rc in range(9):
                wv, bw = wload_in(w_attn_rot, wsrc, [(rc * 128, 128), (1600 + rc * 128, 128)])
                for t in range(NTG):
                    s1_, s2_ = s_rot.next(), s_rot.next()
                    proj_fm(wv, bw, 0, t, s1_)
                    proj_fm(wv, bw, 128, t, s2_)
                    t1, bt1 = tmp_rot.next()
                    t2, bt2 = tmp_rot.next()
                    vtt(t1[:, :], ps[s1_][:, :], Ctab[:, tgs(t)], ALU.mult, [B_ps[s1_], B_tab], [bt1])
                    vtt(t2[:, :], ps[s2_][:, :], Stab[:, tgs(t)], ALU.mult, [B_ps[s2_], B_tab], [bt2])
                    if rc < 6:
                        vtt(qa[:, rc, tgs(t)], t1[:, :], t2[:, :], ALU.add, [bt1, bt2], [B_qa[rc][t]])
                    else:
                        vtt(KT2[:, rc - 6, tgs(t)], t1[:, :], t2[:, :], ALU.add, [bt1, bt2], [B_KT2[rc - 6][t]])
            wv, bw = wload_in(w_attn_rot, wsrc, [(1152, 448)])
            for b in range(NTB):
                si = s_rot.next()
                for k in range(NCH):
                    mm(ps[si][:, 0:192], xb[:, k, tbs(b)], wv[:, k, 0:192], k == 0, k == NCH - 1,
                       [bw, B_xb[k][b // 4]], [B_ps[si]])
                act(V[:, b, :], ps[si][:, 0:192], AF.Identity, [B_ps[si]], [B_V[b]])
            for c in (6, 7):
                for t in range(NTG):
                    si = s_rot.next()
                    proj_fm(wv, bw, 192 + (c - 6) * 128, t, si)
                    act(qa[:, c, tgs(t)], ps[si][:, :], AF.Identity, [B_ps[si]], [B_qa[c][t]])
            _chk("B%d" % l)
            def swa_item(h, t, qb, po, pd):
                g = h // 4
                c = h // 2
                r = h % 2
                rows = slice(r * 64, (r + 1) * 64)
                lo = 0 if qb > 0 else 128
                qcols = slice((qb % 4) * 128, (qb % 4 + 1) * 128)
                st = {}

                def s1():
                    si = s_rot.next()
                    if qb > 0:
                        mm(ps[si][:, 0:128], KT2[rows, g, tbs(qb - 1)], qa[rows, c, tbs(qb)], True, True,
                           [B_KT2[g][(qb - 1) // 4], B_qa[c][t]], [B_ps[si]])
                    mm(ps[si][:, 128:256], KT2[rows, g, tbs(qb)], qa[rows, c, tbs(qb)], True, True,
                       [B_KT2[g][t], B_qa[c][t]], [B_ps[si]])
                    p_, bp_ = pt_rot.next()
                    act(p_[:, lo:256], ps[si][:, lo:256], AF.Exp, [B_ps[si]], [bp_], scale=SCALE)
                    vtt(p_[:, lo:256], p_[:, lo:256], swam_bf[:, lo:256], ALU.mult, [bp_, B_tab], [bp_])
                    st["p"] = (p_, bp_)

                def s2():
                    p_, bp_ = st["p"]
                    if qb > 0:
                        mm(ps[po][rows, qcols], V[:, qb - 1, g * 64:(g + 1) * 64], p_[:, 0:128], True, False,
                           [B_V[qb - 1], bp_], [B_ps[po]])
                        mm(ps[pd][rows, qcols], ones_bf[:, 0:64], p_[:, 0:128], True, False, [B_tab, bp_], [B_ps[pd]])
                    mm(ps[po][rows, qcols], V[:, qb, g * 64:(g + 1) * 64], p_[:, 128:256], qb == 0, True,
                       [B_V[qb], bp_], [B_ps[po]])
                    mm(ps[pd][rows, qcols], ones_bf[:, 0:64], p_[:, 128:256], qb == 0, True, [B_tab, bp_], [B_ps[pd]])
                    if qb % 4 == 3:
                        normalize(po, pd, rows, qa[rows, c, tgs(t)], B_qa[c][t],
                                  sink_ap=sinkE[rows, j * 12 + h:j * 12 + h + 1])
                return (s1, s2)

            items = []
            for h in range(12):
                for t in range(NTG):
                    po, pd = acc_rot.next()
                    for qb in range(4 * t, 4 * t + 4):
                        items.append(swa_item(h, t, qb, po, pd))
            run_items(items, 3)
            mem_attention()
        else:
            wsrc = wfox_d[j]
            KTP = [aview(KV_OFF, 2048)]
            B_KTP = [[Buf(f"ktp{i}_{t}", B_phase) for t in range(NTG)] for i in range(1)]
            VP = [aview(KV_OFF + 2048, 4096).rearrange("p (b n) -> p b n", b=NTB)]
            B_VP = [[Buf(f"vp{i}_{t}", B_phase) for t in range(NTG)] for i in range(1)]
            pr.add("dve", lambda e, vp0=VP[0]: e.memset(vp0[:, :, 64:192], 1.0), [], B_VP[0])
            Dall = aview(KV_OFF + 6144, 2048)
            B_Dall = [Buf(f"Dall{t}", B_phase) for t in range(NTG)]
            pr.add("dve", lambda e, Dall=Dall: e.memset(Dall[:, :], 0.0), [], B_Dall)
            DTs = [ln_mean, ln_rstd]
            B_DTs = [B_lnm, B_lnr]
            for t in range(NTG):
                si = s_rot.next()
                for k in range(NCH):
                    mm(ps[si][0:12, :], spm[:, SP_WF + j * 96 + k * 12:SP_WF + j * 96 + (k + 1) * 12],
                       xres[:, k, tgs(t)], k == 0, k == NCH - 1, [B_const, B_xres[k][t]], [B_ps[si]])
                e1, be1 = tmp_rot.next()
                act(e1[0:12, :], ps[si][0:12, :], AF.Exp, [B_ps[si], B_tab], [be1],
                    bias=negbf[0:12, j:j + 1], scale=-1.0 / ALPHA)
                act(e1[0:12, :], e1[0:12, :], AF.Ln, [be1], [be1], bias=1.0)
                dt_, bdt_ = DTs[t % 2], B_DTs[t % 2]
                dtp, bdtp = DTs[(t + 1) % 2], B_DTs[(t + 1) % 2]
                if t == 0:
                    pr.add("dve", lambda e, e1=e1, dt_=dt_: e.tensor_tensor_scan(
                        dt_[0:12, :], ones_bf[0:12, :], e1[0:12, :], 0.0, ALU.mult, ALU.subtract),
                        [be1, B_tab], [bdt_])
                else:
                    pr.add("dve", lambda e, e1=e1, dt_=dt_, dtp=dtp: e.tensor_tensor_scan(
                        dt_[0:12, :], ones_bf[0:12, :], e1[0:12, :], dtp[0:12, 511:512],
                        ALU.mult, ALU.subtract), [be1, B_tab, bdtp], [bdt_])
                d8, bd8 = tmp_rot.next()
                vts(d8[0:12, :], dt_[0:12, :], 8.0, None, ALU.mult, None, [bdt_], [bd8])
                vcopy(Dall[0:12, tgs(t)], d8[0:12, :], [bd8], [B_Dall[t]])
                vcopy(Dall[64:76, tgs(t)], d8[0:12, :], [bd8], [B_Dall[t]])
                si2 = s_rot.next()
                for jb in range(4):
                    tr(ps[si2][:, jb * 12:(jb + 1) * 12], dt_[0:12, tbs(jb)], cp[0:12, CP_ID:CP_ID + 12],
                       [bdt_, B_const], [B_ps[si2]])
                act(negD[:, t * 4:(t + 1) * 4, :], ps[si2][:, 0:48].rearrange("p (b h) -> p b h", b=4), AF.Identity,
                    [B_ps[si2]], [B_negD[t]], scale=-1.0)
            wv, bw = wload_in(w_attn_rot, wsrc, [(2304, 256)])
            for c in (6, 7):
                for t in range(NTG):
                    si = s_rot.next()
                    proj_fm(wv, bw, (c - 6) * 128, t, si)
                    act(qa[:, c, tgs(t)], ps[si][:, :], AF.Identity, [B_ps[si]], [B_qa[c][t]])
            for c in range(6):
                slot = 0
                wv, bw = wload_in(w_attn_rot, wsrc, [(c * 128, 128), (768 + c * 128, 128), (1536 + c * 128, 128)])
                ktp = KTP[slot]
                vp = VP[slot]
                for t in range(NTG):
                    si = s_rot.next()
                    proj_fm(wv, bw, 0, t, si)
                    act(qa[:, c, tgs(t)], ps[si][:, :], AF.Identity, [B_ps[si]], [B_qa[c][t]])
                    si = s_rot.next()
                    proj_fm(wv, bw, 128, t, si)
                    vcopy(ktp[:, tgs(t)], ps[si][:, :], [B_ps[si]], [B_KTP[slot][t]])
                    si = s_rot.next()
                    for jb in range(4):
                        b = t * 4 + jb
                        for k in range(NCH):
                            mm(ps[si][:, jb * 128:(jb + 1) * 128], xb[:, k, tbs(b)], wv[:, k, 256:384], k == 0,
                               k == NCH - 1, [bw, B_xb[k][t]], [B_ps[si]])
                    psv = ps[si][:, :].rearrange("p (b n) -> p b n", b=4)
                    act(vp[:, t * 4:(t + 1) * 4, 0:64], psv[:, :, 0:64], AF.Identity, [B_ps[si]], [B_VP[slot][t]])
                    act(vp[:, t * 4:(t + 1) * 4, 192:256], psv[:, :, 64:128], AF.Identity, [B_ps[si]], [B_VP[slot][t]])
                def fox_item(c, t, kb, pos_, ktp, vp):
                    nkb = 4 * t + 4
                    jd = kb - 4 * t
                    c0 = max(jd, 0) * 128
                    last = (kb == nkb - 1)
                    st = {}
                    rws = [slice(0, 64), slice(64, 128)]

                    def s1():
                        sis = [s_rot.next(), s_rot.next()]
                        for r in range(2):
                            mm(ps[sis[r]][:, c0:512], ktp[rws[r], tbs(kb)], qa[rws[r], c, t * 512 + c0:(t + 1) * 512],
                               True, False, [B_KTP[0][kb // 4], B_qa[c][t]], [B_ps[sis[r]]])
                        for r in range(2):
                            h = 2 * c + r
                            mm(ps[sis[r]][:, c0:512], sel_bf[rws[r], h * 128:(h + 1) * 128],
                               Dall[rws[r], t * 512 + c0:(t + 1) * 512], False, True, [B_tab, B_Dall[t]], [B_ps[sis[r]]])
                        pp_ = []
                        for r in range(2):
                            h = 2 * c + r
                            p_, bp_ = pt_rot.next()
                            act(p_[:, c0:512], ps[sis[r]][:, c0:512], AF.Exp, [B_ps[sis[r]], B_negD[kb // 4]], [bp_],
                                bias=negD[:, kb, h:h + 1], scale=SCALE)
                            if jd >= 0:
                                vtt(p_[:, c0:c0 + 128], p_[:, c0:c0 + 128], tri_bf[:, :], ALU.mult, [bp_, B_tab], [bp_])
                            pp_.append((p_, bp_))
                        st["p"] = pp_

                    def s2():
                        for r in range(2):
                            p_, bp_ = st["p"][r]
                            po = pos_[r]
                            mm(ps[po][:, c0:512], vp[:, kb, r * 128:(r + 1) * 128], p_[:, c0:512], kb == 0, last,
                               [B_VP[0][kb // 4], bp_], [B_ps[po]])
                        if last:
                            for r in range(2):
                                po = pos_[r]
                                rows, drows = rws[r], rws[1 - r]
                                tmpn, btmpn = tmp_rot.next()
                                act(tmpn[drows, :], ps[po][drows, :], AF.Ln, [B_ps[po]], [btmpn])
                                act(tmpn[drows, :], tmpn[drows, :], AF.Exp, [btmpn], [btmpn], scale=-1.0)
                                vcopy(tmpn[rows, :], tmpn[drows, :], [btmpn], [btmpn])
                                vtt(qa[rows, c, tgs(t)], ps[po][rows, :], tmpn[rows, :], ALU.mult, [B_ps[po], btmpn],
                                    [B_qa[c][t]])
                    return (s1, s2)

                items = []
                for t in range(NTG):
                    pos_ = (fox_rot.next(), fox_rot.next())
                    for kb in range(4 * t + 4):
                        items.append(fox_item(c, t, kb, pos_, ktp, vp))
                run_items(items, 1)
            mem_attention()

        if ("attn%d" % l) in tap_d:
            td = tap_d["attn%d" % l]
            G_tap = pr.grp()
            for c in range(NCH):
                for t in range(NTG):
                    tt_, btt = tmp_rot.next()
                    vcopy(tt_[:, :], qa[:, c, tgs(t)], [B_qa[c][t]], [btt])
                    dma_sp(td[c * 128:(c + 1) * 128, tgs(t)], tt_[:, :], G_tap, [btt], [B_out])

        _chk("C%d" % l)
        wo_t = [wload_in(w_attn_rot, wout_d[l], [(hh * 512, 512)]) for hh in range(2)]
        gcol, bcol = (l * 4 + 0) * 8, (l * 4 + 1) * 8
        for t in range(NTG):
            for dc in range(NCH):
                wv, bw = wo_t[dc // 4]
                si = s_rot.next()
                for k in range(NCH):
                    mm(ps[si][:, :], wv[:, k, (dc % 4) * 128:(dc % 4 + 1) * 128], qa[:, k, tgs(t)], k == 0, k == NCH - 1,
                       [bw, B_qa[k][t]], [B_ps[si]])
                vtt(xres[:, dc, tgs(t)], xres[:, dc, tgs(t)], ps[si][:, :], ALU.add, [B_xres[dc][t], B_ps[si]],
                    [B_xres[dc][t]])
            layer_norm(t, gcol, bcol, gcol, bcol)

        if ("x1_%d" % l) in tap_d:
            td = tap_d["x1_%d" % l]
            G_tap = pr.grp()
            for c in range(NCH):
                for t in range(NTG):
                    dma_sp(td[c * 128:(c + 1) * 128, tgs(t)], xres[:, c, tgs(t)], G_tap, [B_xres[c][t]], [B_out])

        _chk("D%d" % l)
        phase_switch()

        def ffn_pass(w_in2d, w_out2d, gate):
            for f0 in range(0, NFC, 4):
                fcn = min(4, NFC - f0)
                wa, bwa = wload_in(w_ffn_rot, w_in2d, [(f0 * 128, fcn * 128)])
                wb, bwb = wload_in(w_ffn_rot, w_in2d, [(DFF + f0 * 128, fcn * 128)])
                wo, bwo = wload_rows(w_ffn_rot, w_out2d, f0, fcn, D)
                for t in range(NTG):
                    for fc in range(fcn):
                        sa_i, sb_i = s_rot.next(), s_rot.next()
                        proj_fm(wa, bwa, fc * 128, t, sa_i)
                        proj_fm(wb, bwb, fc * 128, t, sb_i)
                        sa, bsa = tmp_rot.next()
                        act(sa[:, :], ps[sa_i][:, :], AF.Silu, [B_ps[sa_i]], [bsa])
                        if gate is not None:
                            gt_, bgt_ = gate
                            vtt(sa[:, :], sa[:, :], gt_[:, tgs(t)], ALU.mult, [bsa, bgt_[t]], [bsa])
                        vtt(gp[:, fc, tgs(t)], sa[:, :], ps[sb_i][:, :], ALU.mult, [bsa, B_ps[sb_i]], [B_gp[fc][t]])
                for t in range(NTG):
                    for dc in range(NCH):
                        yi = 4 + (dc % 4)
                        for fc in range(fcn):
                            mm(ps[yi][:, :], wo[:, fc, dc * 128:(dc + 1) * 128], gp[:, fc, tgs(t)], fc == 0, fc == fcn - 1,
                               [bwo, B_gp[fc][t]], [B_ps[yi]])
                        vtt(xres[:, dc, tgs(t)], xres[:, dc, tgs(t)], ps[yi][:, :], ALU.add,
                            [B_xres[dc][t], B_ps[yi]], [B_xres[dc][t]])

        if swa:
            ffn_pass(wfi_d[j], wfo_d[j], None)
        else:
            for b in range(NTB):
                si = s_rot.next()
                for k in range(NCH):
                    mm(ps[si][:, 0:8], xres[:, k, tbs(b)], spm[:, SP_WR + j * 64 + k * 8:SP_WR + j * 64 + (k + 1) * 8],
                       k == 0, k == NCH - 1, [B_xres[k][b // 4], B_const], [B_ps[si]])
                sm, bsm = small_rot.next()
                lg = sm[:, 0:8]
                pr.add("dve", lambda e, lg=lg, si=si: e.scalar_tensor_tensor(
                    lg, ps[si][:, 0:8], 1.0 / ALPHA, spm[:, SP_BR + j * 8:SP_BR + (j + 1) * 8], ALU.mult, ALU.add),
                    [B_ps[si], B_const], [bsm])
                sm2, bsm2 = small_rot.next()
                mx = sm2[:, 0:8]
                pr.add("dve", lambda e, mx=mx, lg=lg: e.max(mx, lg), [bsm], [bsm2])
                dd = sm2[:, 8:9]
                vtt(dd, sm2[:, 1:2], sm2[:, 0:1], ALU.subtract, [bsm2], [bsm2])
                ee = sm2[:, 9:10]
                act(ee, dd, AF.Exp, [bsm2], [bsm2])
                ss = sm2[:, 10:11]
                vts(ss, ee, 1.0, None, ALU.add, None, [bsm2], [bsm2])
                g1 = sm2[:, 11:12]
                vrecip(g1, ss, [bsm2], [bsm2])
                g2 = sm2[:, 12:13]
                vtt(g2, ee, g1, ALU.mult, [bsm2], [bsm2])
                m1 = sm[:, 8:16]
                vts(m1, lg, sm2[:, 0:1], g1, ALU.is_equal, ALU.mult, [bsm, bsm2], [bsm])
                sm3, bsm3 = small_rot.next()
                m2 = sm3[:, 0:8]
                vts(m2, lg, sm2[:, 1:2], g2, ALU.is_equal, ALU.mult, [bsm, bsm2], [bsm3])
                vtt(Gt[:, b, :], m1, m2, ALU.add, [bsm, bsm3], [B_Gt[b]])
            if ("gate%d" % l) in tap_d:
                td = tap_d["gate%d" % l]
                G_tap = pr.grp()
                for b in range(NTB):
                    dma_sp(td[tbs(b), :], Gt[:, b, :], G_tap, [B_Gt[b]], [B_out])
            for ex in range(8):
                for t in range(NTG):
                    si = s_rot.next()
                    for jb in range(4):
                        b = t * 4 + jb
                        dg, bdg = tmp_rot.next()
                        vts(dg[:, 0:128], ident, Gt[:, b, ex:ex + 1], None, ALU.mult, None, [B_const, B_Gt[b]], [bdg])
                        mm(ps[si][:, jb * 128:(jb + 1) * 128], cp[:, CP_ONES:CP_ONES + 128],
                           dg[:, 0:128], True, True, [B_const, bdg], [B_ps[si]])
                    act(GB[:, tgs(t)], ps[si][:, :], AF.Identity, [B_ps[si]], [B_GB[t]])
                ffn_pass(wei_d[j, ex], weo_d[j, ex], (GB, B_GB))

        gcol, bcol = (l * 4 + 2) * 8, (l * 4 + 3) * 8
        for t in range(NTG):
            layer_norm(t, gcol, bcol, gcol, bcol)
        if ("x2_%d" % l) in tap_d:
            td = tap_d["x2_%d" % l]
            G_tap = pr.grp()
            for c in range(NCH):
                for t in range(NTG):
                    dma_sp(td[c * 128:(c + 1) * 128, tgs(t)], xres[:, c, tgs(t)], G_tap, [B_xres[c][t]], [B_out])

    try:
        _layers()
    except _Stop:
        pass

    phase_switch()
    ps_rot2 = Rot([(ps[i], B_ps[i]) for i in range(8)])
    k_alt = 0
    for b in range(NTB):
        sj = b % 4
        for half in range(2):
            pp, bpp = ps_rot2.next()
            for cc in range(4):
                c = half * 4 + cc
                tr(pp[:, cc * 128:(cc + 1) * 128], xres[:, c, tbs(b)], ident, [B_xres[c][b // 4], B_const], [bpp])
            if k_alt % 2 == 0:
                act(stg[sj][:, half * 512:(half + 1) * 512], pp[:, :], AF.Identity, [bpp], [B_stg[sj]])
            else:
                vcopy(stg[sj][:, half * 512:(half + 1) * 512], pp[:, :], [bpp], [B_stg[sj]])
            k_alt += 1
        dma_sp(out_d[tbs(b), :], stg[sj], G_stg[sj], [B_stg[sj]], [B_outs[b]])
    fin_reads = [B_out] + B_outs + B_stg
    pr.add("sp", lambda e: None, fin_reads, fin_reads)

    pr.emit(nc, es)
    es.close()
    return nc


_CACHE = {}


def _prep_inputs(inp):
    cols = _swa_cols()
    w_swa = np.ascontiguousarray(inp["w_in_swa"][:, :, cols])
    shared = {
        "w_swa": w_swa,
        "w_fox": np.ascontiguousarray(inp["w_in_fox"]),
        "w_kv": np.ascontiguousarray(inp["w_mem_kv"]),
        "w_out": np.ascontiguousarray(inp["w_out"]),
        "w_ffn_in": np.ascontiguousarray(inp["w_ffn_in"]),
        "w_ffn_out": np.ascontiguousarray(inp["w_ffn_out"]),
        "w_exp_in": np.ascontiguousarray(inp["w_exp_in"]),
        "w_exp_out": np.ascontiguousarray(inp["w_exp_out"]),
        "sp": _make_sp(inp),
        "cp": _make_consts()[0],
        "cx": _make_consts()[1],
    }
    return shared


def kernel(**inputs):
    inp = {k: np.asarray(v) for k, v in inputs.items()}
    n = 8
    if "nc" not in _CACHE:
        _CACHE["nc"] = build(4)
    nc = _CACHE["nc"]
    shared = _prep_inputs(inp)
    in_maps = []
    for b in range(n):
        m = dict(shared)
        m["x"] = np.ascontiguousarray(inp["x"][b])
        m["mem"] = np.ascontiguousarray(inp["mem"][b])
        m["pos"] = np.ascontiguousarray(inp["positions"][b].reshape(1, S).astype(np.int32))
        in_maps.append(m)
    res = run_bass_kernel_spmd(nc, in_maps, core_ids=list(range(n)))
    return np.stack([np.asarray(r["out"]) for r in res.results], axis=0).astype(np.float32)
```
